# Optimizing a Trainium2 kernel written in Bass

```python
import jax, jax.numpy as jnp
from jax import lax
import numpy as np

D_MODEL = 2048
BATCH = 2
SEQ = 4096
DEPTH = 1

HEAD_DIM = 128
MOBA_HEADS = 8
MOBA_BLOCK = 256
MOBA_TOPK = 3
DSA_HEADS = 8
IDX_HEADS = 16
IDX_DIM = 64
DSA_TOPK_MAX = 256
D_FF = 4 * D_MODEL
ROPE_THETA = 10000.0
Q_CHUNK = 32
RMS_EPS = 1e-6
MOBA_W = MOBA_HEADS * HEAD_DIM
DSA_W = DSA_HEADS * HEAD_DIM
IN_SIZES = (3 * MOBA_W, 3 * DSA_W, IDX_HEADS * IDX_DIM, IDX_DIM, IDX_HEADS, D_MODEL, D_MODEL)
D_IN = 3 * MOBA_W + 3 * DSA_W + IDX_HEADS * IDX_DIM + IDX_DIM + IDX_HEADS + 2 * D_MODEL

kernel_name = 'hybrid_moba_dsa_gated_block'


def _rmsnorm(x, g):
    xf = x.astype(jnp.float32)
    y = xf * lax.rsqrt(jnp.mean(xf * xf, axis=-1, keepdims=True) + RMS_EPS)
    return (y * g.astype(jnp.float32)).astype(x.dtype)


def _rope(x):
    T, dh = x.shape[1], x.shape[-1]
    half = dh // 2
    inv_freq = jnp.power(ROPE_THETA, -jnp.arange(half, dtype=jnp.float32) / half)
    ang = jnp.arange(T, dtype=jnp.float32)[:, None] * inv_freq[None, :]
    cos = jnp.cos(ang)[None, :, None, :]
    sin = jnp.sin(ang)[None, :, None, :]
    xf = x.astype(jnp.float32)
    x1, x2 = xf[..., :half], xf[..., half:]
    return jnp.concatenate([x1 * cos - x2 * sin, x2 * cos + x1 * sin], axis=-1).astype(x.dtype)


def _moba_attention(q, k, v):
    B, T, H, D = q.shape
    scale = D ** -0.5
    nb = -(-T // MOBA_BLOCK)
    pad = nb * MOBA_BLOCK - T
    kpad = jnp.pad(k, ((0, 0), (0, pad), (0, 0), (0, 0)))
    vpad = jnp.pad(v, ((0, 0), (0, pad), (0, 0), (0, 0)))
    kb = kpad.reshape(B, nb, MOBA_BLOCK, H, D)
    vb = vpad.reshape(B, nb, MOBA_BLOCK, H, D)
    k_mean = jnp.mean(kb.astype(jnp.float32), axis=2)
    gate = jnp.einsum('bthd,bnhd->bthn', q.astype(jnp.float32), k_mean)
    qblk = jnp.arange(T) // MOBA_BLOCK
    past = jnp.arange(nb)[None, :] < qblk[:, None]
    gate = jnp.where(past[None, :, None, :], gate, -jnp.inf)
    n_sel = max(1, min(MOBA_TOPK, nb - 1))
    _, sel_idx = lax.top_k(gate, n_sel)
    sel_ok = sel_idx < qblk[None, :, None, None]
    kbt = jnp.transpose(kb, (0, 3, 1, 2, 4))
    vbt = jnp.transpose(vb, (0, 3, 1, 2, 4))
    b_ix = jnp.arange(B)[:, None, None, None]
    h_ix = jnp.arange(H)[None, None, :, None]

    def chunk(start):
        qc = lax.dynamic_slice_in_dim(q, start, Q_CHUNK, axis=1)
        idx = lax.dynamic_slice_in_dim(sel_idx, start, Q_CHUNK, axis=1)
        ok = lax.dynamic_slice_in_dim(sel_ok, start, Q_CHUNK, axis=1)
        kg = kbt[b_ix, h_ix, idx]
        vg = vbt[b_ix, h_ix, idx]
        s_sel = jnp.einsum('bchd,bchsjd->bchsj', qc, kg).astype(jnp.float32) * scale
        s_sel = jnp.where(ok[..., None], s_sel, -jnp.inf).reshape(B, Q_CHUNK, H, n_sel * MOBA_BLOCK)
        blk_start = (start // MOBA_BLOCK) * MOBA_BLOCK
        ko = lax.dynamic_slice_in_dim(kpad, blk_start, MOBA_BLOCK, axis=1)
        vo = lax.dynamic_slice_in_dim(vpad, blk_start, MOBA_BLOCK, axis=1)
        s_own = jnp.einsum('bchd,bjhd->bchj', qc, ko).astype(jnp.float32) * scale
        qpos = start + jnp.arange(Q_CHUNK)
        kpos = blk_start + jnp.arange(MOBA_BLOCK)
        causal = kpos[None, :] <= qpos[:, None]
        s_own = jnp.where(causal[None, :, None, :], s_own, -jnp.inf)
        p = jax.nn.softmax(jnp.concatenate([s_sel, s_own], axis=-1), axis=-1).astype(v.dtype)
        p_sel = p[..., :n_sel * MOBA_BLOCK].reshape(B, Q_CHUNK, H, n_sel, MOBA_BLOCK)
        p_own = p[..., n_sel * MOBA_BLOCK:]
        return (jnp.einsum('bchsj,bchsjd->bchd', p_sel, vg)
                + jnp.einsum('bchj,bjhd->bchd', p_own, vo))

    starts = jnp.arange(T // Q_CHUNK, dtype=jnp.int32) * Q_CHUNK
    outs = lax.map(chunk, starts)
    return jnp.moveaxis(outs, 0, 1).reshape(B, T, H, D)


def _dsa_attention(q, k, v, q_idx, k_idx, w_idx):
    B, T, H, D = q.shape
    scale = D ** -0.5
    idx_scale = (IDX_DIM ** -0.5) * (IDX_HEADS ** -0.5)
    topk = min(DSA_TOPK_MAX, T // 4)
    kidx_f = k_idx.astype(jnp.float32)
    b_ix = jnp.arange(B)[:, None, None]

    def chunk(start):
        qc = lax.dynamic_slice_in_dim(q, start, Q_CHUNK, axis=1)
        qi = lax.dynamic_slice_in_dim(q_idx, start, Q_CHUNK, axis=1).astype(jnp.float32)
        wi = lax.dynamic_slice_in_dim(w_idx, start, Q_CHUNK, axis=1).astype(jnp.float32)
        rel = jax.nn.relu(jnp.einsum('bchi,bsi->bchs', qi, kidx_f))
        score = jnp.einsum('bch,bchs->bcs', wi, rel) * idx_scale
        qpos = start + jnp.arange(Q_CHUNK)
        admissible = jnp.arange(T)[None, :] <= qpos[:, None]
        score = jnp.where(admissible[None], score, -jnp.inf)
        _, idx = lax.top_k(score, topk)
        ok = idx <= qpos[None, :, None]
        kg = k[b_ix, idx]
        vg = v[b_ix, idx]
        s = jnp.einsum('bchd,bckhd->bchk', qc, kg).astype(jnp.float32) * scale
        s = jnp.where(ok[:, :, None, :], s, -jnp.inf)
        p = jax.nn.softmax(s, axis=-1).astype(v.dtype)
        return jnp.einsum('bchk,bckhd->bchd', p, vg)

    starts = jnp.arange(T // Q_CHUNK, dtype=jnp.int32) * Q_CHUNK
    outs = lax.map(chunk, starts)
    return jnp.moveaxis(outs, 0, 1).reshape(B, T, H, D)


def setup_inputs(seed: int = 0) -> dict:
    key = jax.random.key(seed)
    ks = jax.random.split(key, 16)
    f32 = jnp.float32

    def nrm(k, shape, fan_in, mult=1.0):
        return jax.random.normal(k, shape, f32) * (mult * fan_in ** -0.5)

    def gain(k):
        return 1.0 + 0.02 * jax.random.normal(k, (DEPTH, D_MODEL), f32)

    return {
        'x': jax.random.normal(ks[0], (BATCH, SEQ, D_MODEL), f32),
        'c': jax.random.normal(ks[1], (BATCH, D_MODEL), f32),
        'w_ada': nrm(ks[2], (DEPTH, D_MODEL, 6 * D_MODEL), D_MODEL),
        'b_ada': 0.02 * jax.random.normal(ks[3], (DEPTH, 6 * D_MODEL), f32),
        'g_pre_mix': gain(ks[4]),
        'g_post_mix': gain(ks[5]),
        'w_in': nrm(ks[6], (DEPTH, D_MODEL, D_IN), D_MODEL),
        'w_moba_out': nrm(ks[7], (DEPTH, MOBA_W, D_MODEL), MOBA_W),
        'w_dsa_out': nrm(ks[8], (DEPTH, DSA_W, D_MODEL), DSA_W),
        'w_o': nrm(ks[9], (DEPTH, D_MODEL, D_MODEL), D_MODEL),
        'g_pre_ffn': gain(ks[10]),
        'g_post_ffn': gain(ks[11]),
        'w_ff1': nrm(ks[12], (DEPTH, D_MODEL, D_FF), D_MODEL),
        'w_ff2': nrm(ks[13], (DEPTH, D_FF, D_MODEL), D_FF),
    }


def reference(x, c, w_ada, b_ada, g_pre_mix, g_post_mix, w_in, w_moba_out, w_dsa_out, w_o,
              g_pre_ffn, g_post_ffn, w_ff1, w_ff2):
    B, T, _ = x.shape
    cs = jax.nn.silu(c)
    split_pts = [int(v) for v in np.cumsum(IN_SIZES)[:-1]]
    for l in range(DEPTH):
        mod = cs @ w_ada[l] + b_ada[l]
        sh1, sc1, gt1, sh2, sc2, gt2 = [m[:, None, :] for m in jnp.split(mod, 6, axis=-1)]

        h = _rmsnorm(x, g_pre_mix[l]) * (1.0 + sc1) + sh1
        proj = h @ w_in[l]
        p_moba, p_dsa, p_qi, p_ki, p_wi, ga, gb = jnp.split(proj, split_pts, axis=-1)
        qa, ka, va = [t.reshape(B, T, MOBA_HEADS, HEAD_DIM) for t in jnp.split(p_moba, 3, axis=-1)]
        qb, kb, vb = [t.reshape(B, T, DSA_HEADS, HEAD_DIM) for t in jnp.split(p_dsa, 3, axis=-1)]
        q_idx = _rope(p_qi.reshape(B, T, IDX_HEADS, IDX_DIM))
        k_idx = _rope(p_ki[:, :, None, :])[:, :, 0, :]
        ya = _moba_attention(_rope(qa), _rope(ka), va).reshape(B, T, MOBA_W) @ w_moba_out[l]
        yb = _dsa_attention(_rope(qb), _rope(kb), vb, q_idx, k_idx, p_wi).reshape(B, T, DSA_W) @ w_dsa_out[l]
        y = (jax.nn.sigmoid(ga) * ya + jax.nn.sigmoid(gb) * yb) @ w_o[l]
        x = x + gt1 * _rmsnorm(y, g_post_mix[l])

        h = _rmsnorm(x, g_pre_ffn[l]) * (1.0 + sc2) + sh2
        f = jnp.square(jax.nn.relu(h @ w_ff1[l])) @ w_ff2[l]
        x = x + gt2 * _rmsnorm(f, g_post_ffn[l])
    return x
```

```python
import numpy as np
from contextlib import ExitStack
import concourse.bass as bass
import concourse.mybir as mybir
from concourse.bass_utils import run_bass_kernel_spmd

F32 = mybir.dt.float32
BF16 = mybir.dt.bfloat16
U32 = mybir.dt.uint32
ALU = mybir.AluOpType
AF = mybir.ActivationFunctionType
AX = mybir.AxisListType

ENGS = ("pe", "act", "dve", "pool", "sp")


class Op:
    __slots__ = ("eng", "fn", "deps", "sig", "cnt", "sem", "is_dma", "idx", "nosync_same")

    def __init__(self, eng, fn, is_dma):
        self.eng = eng
        self.fn = fn
        self.is_dma = is_dma
        self.deps = []
        self.sig = False
        self.cnt = 0
        self.sem = None
        self.idx = -1
        self.nosync_same = False


class _Rec:
    def __getattr__(self, name):
        def f(*a, **k):
            self.call = (name, a, k)
            return self
        return f


class Prog:
    def __init__(self, nc):
        self.nc = nc
        self.ops = {e: [] for e in ENGS}
        self.last_w = {}
        self.readers = {}
        self.dma_keys = {}
        self.n_ops = 0

    def _collect(self, op, reads, writes):
        deps = {}

        def add_dep(d):
            if d is None or d is op:
                return
            if d.is_dma:
                deps[("dma", id(d))] = d
            else:
                k = ("eng", d.eng)
                if k not in deps or deps[k].idx < d.idx:
                    deps[k] = d

        for r in reads:
            add_dep(self.last_w.get(r))
        for r in writes:
            add_dep(self.last_w.get(r))
            rd = self.readers.get(r)
            if rd:
                for d in rd.values():
                    add_dep(d)
        return deps

    def add(self, eng, fn, reads=(), writes=(), dma_key=None, pe_acc=False):
        is_dma = dma_key is not None
        rec = _Rec()
        fn(rec)
        _name, _a, _k = rec.call
        fn = (lambda eng, _name=_name, _a=_a, _k=_k: getattr(eng, _name)(*_a, **_k))
        writes = list(writes) + [r for r in reads if isinstance(r, tuple) and r[0] == "ps" and r not in writes]
        op = Op(eng, fn, is_dma)
        op.idx = len(self.ops[eng])
        op.nosync_same = pe_acc
        deps = self._collect(op, reads, writes)
        if is_dma:
            ent = self.dma_keys.setdefault(dma_key, [None, None, 0])
            if ent[1] is not None:
                deps[("dma", id(ent[1]))] = ent[1]
            ent[2] += 16
            op.cnt = ent[2]
            op.sem = dma_key
            ent[1] = op
        dl = []
        for d in deps.values():
            if (not d.is_dma) and d.eng == eng and (pe_acc or eng in ("sp", "pe")):
                continue
            dl.append(d)
            d.sig = True
        op.deps = dl
        self.ops[eng].append(op)
        for r in reads:
            rd = self.readers.setdefault(r, {})
            rd[("dma", id(op)) if is_dma else ("eng", eng)] = op
        for r in writes:
            self.last_w[r] = op
            self.readers[r] = {}
        self.n_ops += 1
        return op

    def emit(self, stack, final_waits=()):
        nc = self.nc
        esem = {e: stack.enter_context(nc.semaphore("s_" + e)) for e in ENGS}
        for i, (k, ent) in enumerate(self.dma_keys.items()):
            ent[0] = stack.enter_context(nc.semaphore("d%d" % i))
        for e in ENGS:
            c = 0
            for op in self.ops[e]:
                if op.is_dma:
                    continue
                if op.sig:
                    c += 1
                    op.cnt = c
        block = stack.enter_context(nc.Block())

        def run(e, eng):
            known = {}
            for op in self.ops[e]:
                for d in op.deps:
                    if d.is_dma:
                        s = self.dma_keys[d.sem][0]
                        key = ("d", d.sem)
                    else:
                        s = esem[d.eng]
                        key = ("e", d.eng)
                    if known.get(key, 0) >= d.cnt:
                        continue
                    eng.wait_ge(s, d.cnt)
                    known[key] = d.cnt
                if op.fn is None:
                    continue
                ins = op.fn(eng)
                if op.is_dma:
                    ins.then_inc(self.dma_keys[op.sem][0], 16)
                elif op.sig:
                    ins.then_inc(esem[e], 1)
            if e == "sp":
                for k in final_waits:
                    ent = self.dma_keys[k]
                    eng.wait_ge(ent[0], ent[2])

        @block.tensor
        def _(eng):
            run("pe", eng)

        @block.scalar
        def _(eng):
            run("act", eng)

        @block.vector
        def _(eng):
            run("dve", eng)

        @block.gpsimd
        def _(eng):
            run("pool", eng)

        @block.sync
        def _(eng):
            run("sp", eng)

    def barrier(self):
        lasts = []
        for e in ENGS:
            for op in reversed(self.ops[e]):
                if not op.is_dma and op.fn is not None:
                    lasts.append(op)
                    break
        for ent in self.dma_keys.values():
            if ent[1] is not None:
                lasts.append(ent[1])
        for e in ENGS:
            op = Op(e, None, False)
            op.idx = len(self.ops[e])
            dl = []
            for d in lasts:
                if (not d.is_dma) and d.eng == e and e in ("pe", "sp"):
                    continue
                d.sig = True
                dl.append(d)
            op.deps = dl
            self.ops[e].append(op)
        self.last_w = {}
        self.readers = {}


D = 2048
T = 4096
NOWN = 1024
DIN = 11344
DFF = 8192
BIGM = 30000.0
SCALE = 128.0 ** -0.5
C_QA, C_KA, C_VA, C_QB, C_KB, C_VB, C_QI, C_KI, C_WI, C_GA, C_GB = (
    0, 1024, 2048, 3072, 4096, 5120, 6144, 7168, 7232, 7248, 9296)
ARENA_BYTES = 176 * 1024
N_BISECT = 24


class Arena:
    def __init__(self, t, nbytes):
        self.t = t
        self.n = nbytes
        self.off = 0

    def reset(self):
        self.off = 0

    def alloc(self, shape, dt):
        n = 1
        for s in shape:
            n *= s
        esz = 4 if dt == F32 else 2
        nb = (n * esz + 63) // 64 * 64
        o = self.off
        self.off += nb
        assert self.off <= self.n, ("arena overflow", self.off, self.n)
        ap = self.t[:, o // 2:(o + n * esz) // 2]
        if dt == F32:
            ap = ap.bitcast(F32)
        if len(shape) == 2:
            ap = ap.rearrange("p (a b) -> p a b", a=shape[0])
        elif len(shape) == 3:
            ap = ap.rearrange("p (a b c) -> p a b c", a=shape[0], b=shape[1])
        return ap


def build(debug=(), stop=None):
    nc = bass.Bass("TRN2", target_bir_lowering=False)
    st = ExitStack()

    def din(name, shape, dt=F32):
        return nc.dram_tensor(name, list(shape), dt, kind="ExternalInput").ap()

    def dscr(name, shape, dt):
        kind = "ExternalOutput" if name in debug else "Internal"
        return nc.dram_tensor(name, list(shape), dt, kind=kind).ap()

    xfull = din("xfull", [T, D])
    xq = din("xq", [NOWN, D])
    cT_d = din("cT", [128, 16])
    w_ada = din("w_ada", [D, 6 * D])
    b_adaT_d = din("b_adaT", [128, 96])
    gT_d = din("gT", [128, 64])
    w_in = din("w_in", [D, DIN])
    w_ki2 = din("w_ki2", [D, 128])
    w_mo = din("w_mo", [1024, D])
    w_do = din("w_do", [1024, D])
    w_o = din("w_o", [D, D])
    w_ff1 = din("w_ff1", [D, DFF])
    w_ff2 = din("w_ff2", [DFF, D])
    ropeF = din("ropeF", [4, 128, T])
    ropeQ = din("ropeQ", [4, 128, NOWN])
    cst = din("cst", [128, 5 * 128 + 8])
    esel_d = din("esel", [16, 16 * 128])
    kpos_d = din("kposB", [128, T])
    out_d = nc.dram_tensor("out", [NOWN, D], F32, kind="ExternalOutput").ap()

    kTa_d = dscr("kTa", [8, 128, T], BF16)
    kTb_d = dscr("kTb", [8, 128, T], BF16)
    va_d = dscr("va", [8, 128, 32, 129], BF16)
    vb_d = dscr("vb", [8, 128, 32, 129], BF16)
    QaT_d = dscr("QaT", [8, 128, NOWN], BF16)
    QbT_d = dscr("QbT", [8, 128, NOWN], BF16)
    QiT_d = dscr("QiT", [8, 128, NOWN], BF16)
    KaTo_d = dscr("KaTo", [8, 128, NOWN], BF16)
    Vao_d = dscr("Vao", [128, 8, 8, 129], BF16)
    hTo_d = dscr("hTo", [128, 16, NOWN], BF16)
    atTa_d = dscr("atTa", [8, 128, NOWN], BF16)
    atTb_d = dscr("atTb", [8, 128, NOWN], BF16)
    gt_d = dscr("gtrow", [2, D], F32)
    x1_d = dscr("x1", [NOWN, D], F32)
    dbg_d = dscr("dbg", [128, 4096], F32)

    arena_t = st.enter_context(nc.sbuf_tensor("sb_arena", [128, ARENA_BYTES // 2], BF16))
    AR = Arena(arena_t, ARENA_BYTES)
    cstF = st.enter_context(nc.sbuf_tensor("sb_cstF", [128, 5 * 128 + 8], F32))
    cstB = st.enter_context(nc.sbuf_tensor("sb_cstB", [128, 4 * 128], BF16))
    eselF = st.enter_context(nc.sbuf_tensor("sb_eselF", [16, 16 * 128], F32))
    eselB = st.enter_context(nc.sbuf_tensor("sb_eselB", [16, 16 * 128], BF16))
    modT = st.enter_context(nc.sbuf_tensor("sb_modT", [128, 96], F32))
    gT = st.enter_context(nc.sbuf_tensor("sb_gT", [128, 64], F32))
    vecs = st.enter_context(nc.sbuf_tensor("sb_vecs", [128, 6 * 16], F32))
    kiT2 = st.enter_context(nc.sbuf_tensor("sb_kiT2", [128, T], BF16))
    ksum = st.enter_context(nc.sbuf_tensor("sb_ksum", [128, 128], F32))
    kmeanT = st.enter_context(nc.sbuf_tensor("sb_kmeanT", [128, 128], BF16))
    wabs = st.enter_context(nc.sbuf_tensor("sb_wabs", [128, 128], F32))
    wsgn = st.enter_context(nc.sbuf_tensor("sb_wsgn", [128, 128], F32))
    small = st.enter_context(nc.sbuf_tensor("sb_small", [128, 64], F32))
    psb = [st.enter_context(nc.psum_tensor("ps%d" % i, [128, 512], F32)) for i in range(8)]

    identF = cstF[:, 0:128]
    blockend = cstF[:, 512:640]
    qposT = cstF[:, 640:648]
    identB = cstB[:, 0:128]
    psw128 = cstB[:, 128:256]
    psw64 = cstB[:, 256:384]
    triB = cstB[:, 384:512]
    G1T, S1T, GT1T = vecs[:, 0:16], vecs[:, 16:32], vecs[:, 32:48]
    G2T, S2T, GT2T = vecs[:, 48:64], vecs[:, 64:80], vecs[:, 80:96]
    neghalf = small[:, 0:1]
    epsc = small[:, 1:2]
    sm_ctr = [2]

    def smcol(n=1):
        if sm_ctr[0] + n > 64:
            sm_ctr[0] = 2
        a = sm_ctr[0]
        sm_ctr[0] += n
        return small[:, a:a + n], ("small", a)

    P = Prog(nc)
    A = P.add

    def PS(i):
        return psb[i][:, :]

    def PSB(i):
        return psb[i][:, :].bitcast(BF16)

    def dma(eng, out, in_, reads, writes, key, nonc=False):
        if nonc:
            return A(eng, lambda e: e.dma_start(out=out, in_=in_, allow_slow_non_contiguous=True),
                     reads=reads, writes=writes, dma_key=key)
        return A(eng, lambda e: e.dma_start(out=out, in_=in_), reads=reads, writes=writes, dma_key=key)

    def mm(out, lhsT, rhs, start, stop, reads, writes):
        return A("pe", lambda e: e.matmul(out, lhsT, rhs, start=start, stop=stop), reads=reads, writes=writes)

    def tr(out, in_, ident, reads, writes):
        return A("pe", lambda e: e.transpose(out, in_, ident), reads=reads, writes=writes)

    dma("sp", cstF[:, :], cst, [], ["cstF"], "c0")
    dma("sp", eselF[:, :], esel_d, [], ["eselF"], "c1")
    dma("sp", gT[:, :], gT_d, [], ["gT"], "c2")
    A("dve", lambda e: e.tensor_copy(out=cstB[:, :], in_=cstF[:, 0:512]), reads=["cstF"], writes=["cstB"])
    A("dve", lambda e: e.tensor_copy(out=eselB[:, :], in_=eselF[:, :]), reads=["eselF"], writes=["eselB"])
    A("dve", lambda e: e.memset(neghalf, -0.5), writes=["neghalf"])
    A("dve", lambda e: e.memset(epsc, 1e-6), reads=["neghalf"], writes=["neghalf"])
    A("dve", lambda e: e.memset(ksum[:, :], 0.0), writes=["ksum"])

    AR.reset()
    cT = AR.alloc([16], F32)
    csT = AR.alloc([16], F32)
    badaT = AR.alloc([96], F32)
    wada = [AR.alloc([16, 512], F32) for _ in range(2)]
    dma("sp", cT, cT_d, [], ["cT"], "c3")
    dma("sp", badaT, b_adaT_d, [], ["badaT"], "c4")
    A("act", lambda e: e.activation(out=csT, in_=cT, func=AF.Silu), reads=["cT"], writes=["csT"])

    def load_wada(c):
        src = w_ada[:, c * 512:(c + 1) * 512].rearrange("(kc p) n -> p kc n", p=128)
        dma("sp", wada[c % 2], src, [], [("wada", c % 2)], ("wada", c % 2))

    load_wada(0)
    for c in range(24):
        if c + 1 < 24:
            load_wada(c + 1)
        wb = wada[c % 2]
        for ci in range(4):
            ct = c * 4 + ci
            for kc in range(16):
                mm(psb[0][:, ct:ct + 1], wb[:, kc, ci * 128:(ci + 1) * 128], csT[:, kc:kc + 1],
                   kc == 0, kc == 15, reads=[("wada", c % 2), "csT"], writes=[("ps", 0)])
    A("dve", lambda e: e.tensor_tensor(out=modT[:, :], in0=psb[0][:, 0:96], in1=badaT, op=ALU.add),
      reads=[("ps", 0), "badaT"], writes=["modT"])
    A("dve", lambda e: e.scalar_tensor_tensor(out=G1T, in0=modT[:, 16:32], scalar=1.0, in1=gT[:, 0:16],
                                               op0=ALU.add, op1=ALU.mult), reads=["modT", "gT"], writes=["vecs"])
    A("dve", lambda e: e.tensor_copy(out=S1T, in_=modT[:, 0:16]), reads=["modT", "vecs"], writes=["vecs"])
    A("dve", lambda e: e.tensor_tensor(out=GT1T, in0=modT[:, 32:48], in1=gT[:, 16:32], op=ALU.mult),
      reads=["modT", "gT", "vecs"], writes=["vecs"])
    A("dve", lambda e: e.scalar_tensor_tensor(out=G2T, in0=modT[:, 64:80], scalar=1.0, in1=gT[:, 32:48],
                                               op0=ALU.add, op1=ALU.mult), reads=["modT", "gT", "vecs"], writes=["vecs"])
    A("dve", lambda e: e.tensor_copy(out=S2T, in_=modT[:, 48:64]), reads=["modT", "vecs"], writes=["vecs"])
    A("dve", lambda e: e.tensor_tensor(out=GT2T, in0=modT[:, 80:96], in1=gT[:, 48:64], op=ALU.mult),
      reads=["modT", "gT", "vecs"], writes=["vecs"])
    dma("sp", gt_d[0].rearrange("(kc p) -> p kc", p=128), GT1T, ["vecs"], ["gt_d"], "c5", nonc=True)
    dma("sp", gt_d[1].rearrange("(kc p) -> p kc", p=128), GT2T, ["vecs"], ["gt_d"], "c5", nonc=True)
    P.barrier()
    if stop == 0:
        return nc, st, P

    class NormCtx:
        pass

    def norm_setup():
        n = NormCtx()
        n.xt = [AR.alloc([D], F32) for _ in range(2)]
        n.xs = [AR.alloc([D], BF16) for _ in range(2)]
        n.junk = AR.alloc([D], BF16)
        n.i = 0
        return n

    def norm_tile(n, src_rows, G, S, dst_fn, dst_res2, pbanks=(0, 1)):
        i = n.i
        n.i += 1
        xt, xs = n.xt[i % 2], n.xs[i % 2]
        rxt, rxs = ("xt", i % 2), ("xs", i % 2)
        dma("sp", xt, src_rows, [], [rxt], rxt)
        ss, rss = smcol()
        vv, rvv = smcol()
        rs, rrs = smcol()
        A("act", lambda e: e.activation(out=n.junk, in_=xt, func=AF.Square, accum_out=ss),
          reads=[rxt], writes=["njunk", rss])
        import os
        KN = int(os.environ.get("KN", "9"))
        if KN < 1:
            return
        A("act", lambda e: e.activation(out=vv, in_=ss, func=AF.Ln, scale=1.0 / D, bias=epsc),
          reads=[rss, "neghalf"], writes=[rvv])
        A("act", lambda e: e.activation(out=rs, in_=vv, func=AF.Exp, scale=-0.5), reads=[rvv], writes=[rrs])
        if KN < 2:
            return
        A("act", lambda e: e.activation(out=xs, in_=xt, func=AF.Copy, scale=rs), reads=[rxt, rrs], writes=[rxs])
        if KN < 3:
            return
        for kc in range(16):
            b = pbanks[kc // 8]
            tr(PSB(b)[:, (kc % 8) * 128:(kc % 8 + 1) * 128], xs[:, kc * 128:(kc + 1) * 128], identB,
               reads=[rxs, "cstB"], writes=[("ps", b)])
        if KN < 4:
            return
        for kc in range(16):
            b = pbanks[kc // 8]
            src = PSB(b)[:, (kc % 8) * 128:(kc % 8 + 1) * 128]
            dst = dst_fn(kc)
            KE = os.environ.get("KE", "mix")
            if (kc // 8 == 0 and KE == "mix") or KE == "dve":
                A("dve", lambda e, src=src, dst=dst, kc=kc: e.tensor_scalar(
                    out=dst, in0=src, scalar1=G[:, kc:kc + 1], scalar2=S[:, kc:kc + 1], op0=ALU.mult, op1=ALU.add),
                  reads=[("ps", b), "vecs"], writes=[dst_res2[0]])
            else:
                A("act", lambda e, src=src, dst=dst, kc=kc: e.activation(
                    out=dst, in_=src, func=AF.Identity, scale=G[:, kc:kc + 1], bias=S[:, kc:kc + 1]),
                  reads=[("ps", b), "vecs"], writes=[dst_res2[1]])

    class WStream:
        def __init__(self, name, nbuf, shape):
            self.name = name
            self.bufs = [AR.alloc(shape, BF16) for _ in range(nbuf)]
            self.n = 0

        def load(self, src_ap, view=None):
            i = self.n % len(self.bufs)
            self.n += 1
            dst = self.bufs[i] if view is None else view(self.bufs[i])
            r = (self.name, i)
            dma("pool", dst, src_ap, [], [r], r)
            return self.bufs[i], r

    def wsrc(w, c0, ncols):
        return w[:, c0:c0 + ncols].rearrange("(kc p) n -> p kc n", p=128)

    class RopeCtx:
        pass

    def rope_setup():
        r = RopeCtx()
        r.xb = [AR.alloc([512], BF16) for _ in range(2)]
        r.t1 = [AR.alloc([512], F32) for _ in range(2)]
        r.t2 = [AR.alloc([512], F32) for _ in range(2)]
        r.i = 0
        return r

    def rope(r, pbank, swbank, cosT, sinT, psw, out_ap, out_res, tab_res):
        i = r.i
        r.i += 1
        xb, t1, t2 = r.xb[i % 2], r.t1[i % 2], r.t2[i % 2]
        import os
        KR = int(os.environ.get("KR", "9"))
        if KR < 1:
            return
        A("act", lambda e: e.activation(out=xb, in_=PS(pbank), func=AF.Copy), reads=[("ps", pbank)], writes=[("rxb", i % 2)])
        if KR < 2:
            return
        mm(PS(swbank), psw, xb, True, True, reads=[("rxb", i % 2), "cstB"], writes=[("ps", swbank)])
        if KR < 3:
            return
        A("dve", lambda e: e.tensor_tensor(out=t1, in0=PS(pbank), in1=cosT, op=ALU.mult),
          reads=[("ps", pbank), tab_res], writes=[("rt1", i % 2)])
        A("dve", lambda e: e.tensor_tensor(out=t2, in0=PS(swbank), in1=sinT, op=ALU.mult),
          reads=[("ps", swbank), tab_res], writes=[("rt2", i % 2)])
        if KR < 4:
            return
        A("dve", lambda e: e.tensor_tensor(out=out_ap, in0=t1, in1=t2, op=ALU.add),
          reads=[("rt1", i % 2), ("rt2", i % 2)], writes=[out_res])

    AR.reset()
    nctx = norm_setup()
    rctx = rope_setup()
    hT = AR.alloc([16, 1024], BF16)
    ws = WStream("w1", 3, [16, 512])
    tabs = AR.alloc([4, 1024], F32)
    kst = [AR.alloc([1024], BF16) for _ in range(2)]
    vst = [AR.alloc([4, 129], BF16) for _ in range(3)]
    for i in range(3):
        A("dve", lambda e, i=i: e.memset(vst[i][:, :, 128:129], 1.0), writes=[("vst", i)])
    kcnt = [0]
    vcnt = [0]
    pk = [0]
    for tg in range(4):
        t0 = tg * 1024
        dma("sp", tabs, ropeF[:, :, t0:t0 + 1024].rearrange("f p t -> p f t"), [], ["tabs"], "tabs")
        for tt in range(8):
            norm_tile(nctx, xfull[t0 + tt * 128:t0 + (tt + 1) * 128, :], G1T, S1T,
                      (lambda kc, tt=tt: hT[:, kc, tt * 128:(tt + 1) * 128]), (("hT", tt, 0), ("hT", tt, 1)))
        hT_res = [("hT", tt, q) for tt in range(8) for q in range(2)]
        if stop == 1:
            P.barrier()
            return nc, st, P
        kjobs = []
        kjobs.append((wsrc(w_in, C_KA, 512), 512, [(ci, "a", ci) for ci in range(4)]))
        kjobs.append((wsrc(w_in, C_KA + 512, 512), 512, [(ci, "a", 4 + ci) for ci in range(4)]))
        kjobs.append((wsrc(w_in, C_KB, 512), 512, [(ci, "b", ci) for ci in range(4)]))
        kjobs.append((wsrc(w_in, C_KB + 512, 512), 512, [(ci, "b", 4 + ci) for ci in range(4)]))
        kjobs.append((wsrc(w_ki2, 0, 128), 128, [(0, "i", 0)]))
        for (src, ncols, tiles) in kjobs:
            wb, wr = ws.load(src, view=(lambda b, ncols=ncols: b[:, :, 0:ncols]))
            for (ci, kind, head) in tiles:
                ks = kst[kcnt[0] % 2]
                ksr = ("kst", kcnt[0] % 2)
                kcnt[0] += 1
                for half in range(2):
                    pb = 2 + (pk[0] % 2)
                    sb_ = 4 + (pk[0] % 2)
                    pk[0] += 1
                    for kc in range(16):
                        mm(PS(pb), wb[:, kc, ci * 128:(ci + 1) * 128], hT[:, kc, half * 512:(half + 1) * 512],
                           kc == 0, kc == 15, reads=[wr] + hT_res[half * 8:half * 8 + 8], writes=[("ps", pb)])
                    if kind == "i":
                        cosT, sinT, psw = tabs[:, 2, half * 512:(half + 1) * 512], tabs[:, 3, half * 512:(half + 1) * 512], psw64
                        oap, ores = kiT2[:, t0 + half * 512:t0 + (half + 1) * 512], "kiT2"
                    else:
                        cosT, sinT, psw = tabs[:, 0, half * 512:(half + 1) * 512], tabs[:, 1, half * 512:(half + 1) * 512], psw128
                        oap, ores = ks[:, half * 512:(half + 1) * 512], ksr
                    rope(rctx, pb, sb_, cosT, sinT, psw, oap, ores, "tabs")
                if kind == "a":
                    A("dve", lambda e, ks=ks, head=head, tg=tg: e.tensor_reduce(
                        out=ksum[:, head * 16 + tg * 4:head * 16 + tg * 4 + 4],
                        in_=ks.rearrange("p (n j) -> p n j", j=256), axis=AX.X, op=ALU.add),
                      reads=[ksr], writes=["ksum"])
                if kind in ("a", "b"):
                    dst = (kTa_d if kind == "a" else kTb_d)[head, :, t0:t0 + 1024]
                    dma("sp", dst, ks, [ksr], [("kT", kind, head)], ("kstore", kcnt[0] % 2))
        if stop == 2:
            P.barrier()
            return nc, st, P
        for (c0, vd, hb) in ((C_VA, va_d, 0), (C_VA + 512, va_d, 4), (C_VB, vb_d, 0), (C_VB + 512, vb_d, 4)):
            wb, wr = ws.load(wsrc(w_in, c0, 512))
            for tt in range(8):
                pb = 6 + (vcnt[0] % 2)
                vs = vst[vcnt[0] % 3]
                vsr = ("vst", vcnt[0] % 3)
                vcnt[0] += 1
                for kc in range(16):
                    mm(PS(pb), hT[:, kc, tt * 128:(tt + 1) * 128], wb[:, kc, :], kc == 0, kc == 15,
                       reads=[wr, ("hT", tt, 0), ("hT", tt, 1)], writes=[("ps", pb)])
                A("act", lambda e, vs=vs, pb=pb: e.activation(
                    out=vs[:, :, 0:128], in_=PS(pb).rearrange("p (h d) -> p h d", h=4), func=AF.Copy),
                  reads=[("ps", pb)], writes=[vsr])
                gtile = tg * 8 + tt
                dma("sp", vd[hb:hb + 4, :, gtile, :].rearrange("h p c -> p h c"), vs, [vsr],
                    [("vd", id(vd), hb, gtile)], ("vstore", vcnt[0] % 3))
        if stop == 3:
            P.barrier()
            return nc, st, P
    A("dve", lambda e: e.tensor_scalar(out=kmeanT[:, :], in0=ksum[:, :], scalar1=1.0 / 256.0, scalar2=None, op0=ALU.mult),
      reads=["ksum"], writes=["kmeanT"])
    P.barrier()
    if stop == 4:
        return nc, st, P

    AR.reset()
    nctx = norm_setup()
    rctx = rope_setup()
    hT = AR.alloc([16, 1024], BF16)
    ws = WStream("w2", 3, [16, 512])
    tabs = AR.alloc([4, 1024], F32)
    kst = [AR.alloc([1024], BF16) for _ in range(2)]
    vst = [AR.alloc([4, 129], BF16) for _ in range(3)]
    wwi = AR.alloc([16, 16], BF16)
    for i in range(3):
        A("dve", lambda e, i=i: e.memset(vst[i][:, :, 128:129], 1.0), writes=[("vst", i)])
    dma("sp", tabs, ropeQ.rearrange("f p t -> p f t"), [], ["tabs"], "tabs")
    dma("pool", wwi, w_in[:, C_WI:C_WI + 16].rearrange("(kc p) n -> p kc n", p=128), [], ["wwi"], "wwi")
    for tt in range(8):
        norm_tile(nctx, xq[tt * 128:(tt + 1) * 128, :], G1T, S1T,
                  (lambda kc, tt=tt: hT[:, kc, tt * 128:(tt + 1) * 128]), (("hT", tt, 0), ("hT", tt, 1)))
    hT_res = [("hT", tt, q) for tt in range(8) for q in range(2)]
    dma("sp", hTo_d, hT, hT_res, ["hTo_d"], "hTo")
    kcnt = [0]
    pk = [0]
    for (c0, dst_d, r64) in ((C_QA, QaT_d, False), (C_KA, KaTo_d, False), (C_QB, QbT_d, False), (C_QI, QiT_d, True)):
        for ch in range(2):
            wb, wr = ws.load(wsrc(w_in, c0 + ch * 512, 512))
            for ci in range(4):
                head = ch * 4 + ci
                ks = kst[kcnt[0] % 2]
                ksr = ("kst", kcnt[0] % 2)
                kcnt[0] += 1
                for half in range(2):
                    pb = 2 + (pk[0] % 2)
                    sb_ = 4 + (pk[0] % 2)
                    pk[0] += 1
                    for kc in range(16):
                        mm(PS(pb), wb[:, kc, ci * 128:(ci + 1) * 128], hT[:, kc, half * 512:(half + 1) * 512],
                           kc == 0, kc == 15, reads=[wr] + hT_res[half * 8:half * 8 + 8], writes=[("ps", pb)])
                    sl = slice(half * 512, (half + 1) * 512)
                    if r64:
                        rope(rctx, pb, sb_, tabs[:, 2, sl], tabs[:, 3, sl], psw64, ks[:, sl], ksr, "tabs")
                    else:
                        rope(rctx, pb, sb_, tabs[:, 0, sl], tabs[:, 1, sl], psw128, ks[:, sl], ksr, "tabs")
                dma("sp", dst_d[head, :, :], ks, [ksr], [("qd", c0, head)], ("kstore", kcnt[0] % 2))
    vcnt = [0]
    for ch in range(2):
        wb, wr = ws.load(wsrc(w_in, C_VA + ch * 512, 512))
        for tt in range(8):
            pb = 6 + (vcnt[0] % 2)
            vs = vst[vcnt[0] % 3]
            vsr = ("vst", vcnt[0] % 3)
            vcnt[0] += 1
            for kc in range(16):
                mm(PS(pb), hT[:, kc, tt * 128:(tt + 1) * 128], wb[:, kc, :], kc == 0, kc == 15,
                   reads=[wr, ("hT", tt, 0), ("hT", tt, 1)], writes=[("ps", pb)])
            A("act", lambda e, vs=vs, pb=pb: e.activation(
                out=vs[:, :, 0:128], in_=PS(pb).rearrange("p (h d) -> p h d", h=4), func=AF.Copy),
              reads=[("ps", pb)], writes=[vsr])
            dma("sp", Vao_d[:, tt, ch * 4:ch * 4 + 4, :], vs, [vsr], [("vao", tt, ch)], ("vstore", vcnt[0] % 3))
    for tt in range(8):
        pb = 6 + (tt % 2)
        for kc in range(16):
            mm(psb[pb][:, 0:16], hT[:, kc, tt * 128:(tt + 1) * 128], wwi[:, kc, :], kc == 0, kc == 15,
               reads=["wwi", ("hT", tt, 0), ("hT", tt, 1)], writes=[("ps", pb)])
        A("act", lambda e, tt=tt, pb=pb: e.activation(out=wabs[:, tt * 16:(tt + 1) * 16], in_=psb[pb][:, 0:16], func=AF.Abs),
          reads=[("ps", pb)], writes=["wabs"])
        A("act", lambda e, tt=tt, pb=pb: e.activation(out=wsgn[:, tt * 16:(tt + 1) * 16], in_=psb[pb][:, 0:16], func=AF.Sign),
          reads=[("ps", pb)], writes=["wsgn"])
    P.barrier()
    if stop == 5:
        return nc, st, P

    def finalize_heads(h, g, asb, stg, dst_d, ia):
        for qs in range(4):
            rd, rrd = smcol()
            A("dve", lambda e, rd=rd, qs=qs: e.reciprocal(out=rd, in_=psb[2 + qs][:, 128:129]),
              reads=[("ps", 2 + qs)], writes=[rrd])
            A("act", lambda e, rd=rd, qs=qs: e.activation(out=asb[:, qs, :], in_=psb[2 + qs][:, 0:128], func=AF.Copy, scale=rd),
              reads=[("ps", 2 + qs), rrd], writes=[("asb", ia, qs)])
        for qs in range(4):
            tr(PSB(7)[:, qs * 128:(qs + 1) * 128], asb[:, qs, :], identB, reads=[("asb", ia, qs), "cstB"], writes=[("ps", 7)])
        A("dve", lambda e: e.tensor_copy(out=stg, in_=PSB(7)[:, 0:512]), reads=[("ps", 7)], writes=[("stg", ia)])
        dma("sp", dst_d[h, :, g * 512:(g + 1) * 512], stg, [("stg", ia)], [("atd", id(dst_d), h, g)], ("atst", ia))

    for g in range(2):
        NKT = 16 if g == 0 else 32
        NK = NKT * 128
        AR.reset()
        QiG = AR.alloc([8, 512], BF16)
        kposB = AR.alloc([NK], F32)
        score = AR.alloc([NK], F32)
        cm = AR.alloc([NK], F32)
        junk = AR.alloc([NK], BF16)
        m01 = AR.alloc([NK], BF16)
        maskT = AR.alloc([NKT, 512], BF16)
        bst = AR.alloc([8], F32)
        QbG = AR.alloc([8, 512], BF16)
        Kh = [AR.alloc([NK], BF16) for _ in range(2)]
        Vh = [AR.alloc([NKT, 129], BF16) for _ in range(2)]
        esb = [AR.alloc([512], BF16) for _ in range(2)]
        pT = [AR.alloc([512], BF16) for _ in range(2)]
        asb = [AR.alloc([4, 128], BF16) for _ in range(2)]
        stg = [AR.alloc([512], BF16) for _ in range(2)]
        dma("sp", QiG, QiT_d[:, :, g * 512:(g + 1) * 512].rearrange("h p t -> p h t"), [], ["QiG"], "QiG")
        dma("sp", kposB, kpos_d[:, 0:NK], [], ["kposB"], "kposB")
        dma("sp", QbG, QbT_d[:, :, g * 512:(g + 1) * 512].rearrange("h p t -> p h t"), [], ["QbG"], "QbG")
        cnt = [0]
        for qt in range(4):
            Tq = 4 * g + qt
            for c in range(NKT // 4):
                scr = ("score", c)
                sc = score[:, c * 512:(c + 1) * 512]
                for h in range(16):
                    half, pair = h % 2, h // 2
                    bs_, br_ = cnt[0] % 2, 2 + (cnt[0] % 2)
                    cnt[0] += 1
                    mm(PS(bs_), QiG[64 * half:64 * half + 64, pair, qt * 128:(qt + 1) * 128],
                       kiT2[64 * half:64 * half + 64, c * 512:(c + 1) * 512], True, True,
                       reads=["QiG", "kiT2"], writes=[("ps", bs_)])
                    col = Tq * 16 + h
                    A("act", lambda e, bs_=bs_, br_=br_, col=col: e.activation(
                        out=PS(br_), in_=PS(bs_), func=AF.Relu, scale=wabs[:, col:col + 1]),
                      reads=[("ps", bs_), "wabs"], writes=[("ps", br_)])
                    if h == 0:
                        A("dve", lambda e, br_=br_, col=col, sc=sc: e.tensor_scalar(
                            out=sc, in0=PS(br_), scalar1=wsgn[:, col:col + 1], scalar2=None, op0=ALU.mult),
                          reads=[("ps", br_), "wsgn"], writes=[scr])
                    else:
                        A("dve", lambda e, br_=br_, col=col, sc=sc: e.scalar_tensor_tensor(
                            out=sc, in0=PS(br_), scalar=wsgn[:, col:col + 1], in1=sc, op0=ALU.mult, op1=ALU.add),
                          reads=[("ps", br_), "wsgn", scr], writes=[scr])
            allsc = [("score", c) for c in range(NKT // 4)]
            rmx, rmn, w0, lo, mid, cntc, tt_ = [bst[:, i:i + 1] for i in range(7)]
            B_ = lambda i: ("bst", i)
            A("dve", lambda e: e.tensor_reduce(out=rmx, in_=score, axis=AX.X, op=ALU.max), reads=allsc, writes=[B_(0)])
            A("dve", lambda e: e.tensor_reduce(out=rmn, in_=score, axis=AX.X, op=ALU.min), reads=allsc, writes=[B_(1)])
            A("dve", lambda e: e.tensor_tensor(out=w0, in0=rmx, in1=rmn, op=ALU.subtract), reads=[B_(0), B_(1)], writes=[B_(2)])
            A("dve", lambda e: e.tensor_scalar(out=w0, in0=w0, scalar1=1.001, scalar2=1e-20, op0=ALU.mult, op1=ALU.add),
              reads=[B_(2)], writes=[B_(2)])
            A("dve", lambda e: e.tensor_copy(out=lo, in_=rmn), reads=[B_(1)], writes=[B_(3)])
            A("dve", lambda e, Tq=Tq: e.tensor_scalar(out=cm, in0=kposB, scalar1=qposT[:, Tq:Tq + 1], scalar2=-1.0e9,
                                                      op0=ALU.is_gt, op1=ALU.mult), reads=["kposB", "cstF"], writes=["cm"])
            A("dve", lambda e: e.tensor_tensor(out=score, in0=score, in1=cm, op=ALU.add), reads=allsc + ["cm"], writes=allsc)
            for it in range(1, N_BISECT + 1):
                f = 2.0 ** (-it)
                A("dve", lambda e, f=f: e.tensor_scalar(out=mid, in0=w0, scalar1=f, scalar2=lo, op0=ALU.mult, op1=ALU.add),
                  reads=[B_(2), B_(3)], writes=[B_(4)])
                A("dve", lambda e: e.tensor_scalar(out=junk, in0=score, scalar1=mid, scalar2=None, op0=ALU.is_ge, op1=ALU.add,
                                                   accum_out=cntc), reads=allsc + [B_(4)], writes=["junk", B_(5)])
                A("dve", lambda e: e.tensor_scalar(out=tt_, in0=cntc, scalar1=255.5, scalar2=w0, op0=ALU.is_ge, op1=ALU.mult),
                  reads=[B_(5), B_(2)], writes=[B_(6)])
                A("dve", lambda e, f=f: e.tensor_scalar(out=lo, in0=tt_, scalar1=f, scalar2=lo, op0=ALU.mult, op1=ALU.add),
                  reads=[B_(6), B_(3)], writes=[B_(3)])
            A("dve", lambda e: e.tensor_scalar(out=m01, in0=score, scalar1=lo, scalar2=None, op0=ALU.is_ge),
              reads=allsc + [B_(3)], writes=["m01"])
            if stop == 60 or (stop == 61 and g == 1):
                dma("sp", dbg_d[:, 0:NK], score, allsc, ["dbg0"], "dbg0")
                dma("sp", dbg_d[:, 2048:2048 + NK // 2].bitcast(BF16), m01, ["m01"], ["dbg4"], "dbg4")
                dma("sp", dbg_d[:, 4000:4008], bst, [B_(i) for i in range(7)], ["dbg1"], "dbg1")
                dma("sp", dbg_d[:, 4010:4026], wabs[:, 0:16], [], ["dbg2"], "dbg2")
                dma("sp", dbg_d[:, 4030:4046], wsgn[:, 0:16], [], ["dbg3"], "dbg3")
                P.barrier()
                return nc, st, P
            for k8 in range(NKT // 8):
                bank = 4 + (k8 % 2)
                for j in range(8):
                    kt = k8 * 8 + j
                    tr(PSB(bank)[:, j * 128:(j + 1) * 128], m01[:, kt * 128:(kt + 1) * 128], identB,
                       reads=["m01", "cstB"], writes=[("ps", bank)])
                A("act", lambda e, bank=bank, k8=k8, qt=qt: e.activation(
                    out=maskT[:, k8 * 8:(k8 + 1) * 8, qt * 128:(qt + 1) * 128],
                    in_=PSB(bank).rearrange("p (j q) -> p j q", j=8), func=AF.Copy),
                  reads=[("ps", bank)], writes=[("maskT", qt)])
        maskT_res = [("maskT", qt) for qt in range(4)]
        if stop == 6:
            dma("sp", dbg_d[:, 0:NKT * 256].bitcast(BF16) if False else x1_d[0:128, :].bitcast(BF16)[:, 0:4096].rearrange("p (a b) -> p a b", a=8)[:, :, :],
                maskT[:, 0:8, :], maskT_res, ["dbgm"], "dbgm") if False else None
            P.barrier()
            return nc, st, P

        if stop == 61:
            P.barrier()
            continue
        def loadKV(h, kd, vd, pref):
            i = h % 2
            dma("sp", Kh[i], kd[h, :, 0:NK], [], [(pref + "K", i)], (pref + "K", i))
            dma("sp", Vh[i], vd[h, :, 0:NKT, :], [], [(pref + "V", i)], (pref + "V", i))

        loadKV(0, kTb_d, vb_d, "b")
        for h in range(8):
            if h + 1 < 8:
                loadKV(h + 1, kTb_d, vb_d, "b")
            i = h % 2
            for kt in range(NKT):
                bs_ = kt % 2
                mm(PS(bs_), Kh[i][:, kt * 128:(kt + 1) * 128], QbG[:, h, :], True, True,
                   reads=[("bK", i), "QbG"], writes=[("ps", bs_)])
                A("act", lambda e, bs_=bs_, kt=kt: e.activation(out=esb[kt % 2], in_=PS(bs_), func=AF.Exp, scale=SCALE),
                  reads=[("ps", bs_)], writes=[("esb", kt % 2)])
                A("dve", lambda e, kt=kt: e.tensor_tensor(out=pT[kt % 2], in0=esb[kt % 2], in1=maskT[:, kt, :], op=ALU.mult),
                  reads=[("esb", kt % 2)] + maskT_res, writes=[("pT", kt % 2)])
                for qs in range(4):
                    mm(psb[2 + qs][:, 0:129], pT[kt % 2][:, qs * 128:(qs + 1) * 128], Vh[i][:, kt, :], kt == 0, kt == NKT - 1,
                       reads=[("pT", kt % 2), ("bV", i)], writes=[("ps", 2 + qs)])
            finalize_heads(h, g, asb[h % 2], stg[h % 2], atTb_d, h % 2)
        P.barrier()
    if stop == 7:
        return nc, st, P

    AR.reset()
    QaT = AR.alloc([8, 1024], BF16)
    KaTo = AR.alloc([8, 1024], BF16)
    Vao = AR.alloc([8, 8, 129], BF16)
    MT = AR.alloc([8, 1024], BF16)
    gsc = AR.alloc([6, 128], F32)
    m8 = AR.alloc([64], F32)
    Mq = AR.alloc([128], BF16)
    Kh = [AR.alloc([4096], BF16) for _ in range(2)]
    Vh = [AR.alloc([32, 129], BF16) for _ in range(2)]
    pT = [AR.alloc([512], BF16) for _ in range(2)]
    eo = [AR.alloc([128], BF16) for _ in range(2)]
    asb = [AR.alloc([4, 128], BF16) for _ in range(2)]
    stg = [AR.alloc([512], BF16) for _ in range(2)]
    dma("sp", QaT, QaT_d.rearrange("h p t -> p h t"), [], ["QaT"], "QaT")
    dma("sp", KaTo, KaTo_d.rearrange("h p t -> p h t"), [], ["KaTo"], "KaTo")
    dma("sp", Vao, Vao_d, [], ["Vao"], "Vao")
    pastm, negm, gm, sel = gsc[:, 0, :], gsc[:, 1, :], gsc[:, 2, :], gsc[:, 3, :]
    for Tq in range(8):
        for h in range(8):
            mm(psb[7][:, h * 16:(h + 1) * 16], QaT[:, h, Tq * 128:(Tq + 1) * 128], kmeanT[:, h * 16:(h + 1) * 16], True, True,
               reads=["QaT", "kmeanT"], writes=[("ps", 7)])
        A("dve", lambda e, Tq=Tq: e.tensor_scalar(out=pastm, in0=blockend, scalar1=qposT[:, Tq:Tq + 1], scalar2=None, op0=ALU.is_le),
          reads=["cstF"], writes=["pastm"])
        A("dve", lambda e: e.tensor_scalar(out=negm, in0=pastm, scalar1=1.0, scalar2=BIGM, op0=ALU.subtract, op1=ALU.mult),
          reads=["pastm"], writes=["negm"])
        A("dve", lambda e: e.tensor_tensor(out=gm, in0=psb[7][:, 0:128], in1=negm, op=ALU.add),
          reads=[("ps", 7), "negm"], writes=["gm"])
        for h in range(8):
            A("dve", lambda e, h=h: e.max(out=m8[:, h * 8:(h + 1) * 8], in_=gm[:, h * 16:(h + 1) * 16]),
              reads=["gm"], writes=[("m8", h)])
        for h in range(8):
            A("dve", lambda e, h=h: e.tensor_scalar(out=sel[:, h * 16:(h + 1) * 16], in0=gm[:, h * 16:(h + 1) * 16],
                                                     scalar1=m8[:, h * 8 + 2:h * 8 + 3], scalar2=None, op0=ALU.is_ge),
              reads=["gm", ("m8", h)], writes=[("sel", h)])
        A("dve", lambda e: e.tensor_tensor(out=sel, in0=sel, in1=pastm, op=ALU.mult),
          reads=[("sel", h) for h in range(8)] + ["pastm"], writes=[("sel", h) for h in range(8)])
        A("dve", lambda e: e.tensor_scalar(out=Mq, in0=sel, scalar1=1.0, scalar2=BIGM, op0=ALU.subtract, op1=ALU.mult),
          reads=[("sel", h) for h in range(8)], writes=["Mq"])
        for h in range(8):
            tr(PSB(6)[0:16, h * 128:(h + 1) * 128], Mq[:, h * 16:(h + 1) * 16], identB, reads=["Mq", "cstB"], writes=[("ps", 6)])
        A("act", lambda e, Tq=Tq: e.activation(out=MT[0:16, :, Tq * 128:(Tq + 1) * 128],
                                               in_=PSB(6)[0:16, :].rearrange("p (h q) -> p h q", h=8), func=AF.Copy),
          reads=[("ps", 6)], writes=[("MT", Tq)])
    for g in range(2):
        NKT = 16 if g == 0 else 32
        NK = NKT * 128
        MT_res = [("MT", 4 * g + q) for q in range(4)]

        def loadKVa(h):
            i = h % 2
            dma("sp", Kh[i][:, 0:NK], kTa_d[h, :, 0:NK], [], [("aK", i)], ("aK", i))
            dma("sp", Vh[i][:, 0:NKT, :], va_d[h, :, 0:NKT, :], [], [("aV", i)], ("aV", i))

        loadKVa(0)
        ecnt = [0]
        for h in range(8):
            if h + 1 < 8:
                loadKVa(h + 1)
            i = h % 2
            for kt in range(NKT):
                bs_ = kt % 2
                mm(PS(bs_), Kh[i][:, kt * 128:(kt + 1) * 128], QaT[:, h, g * 512:(g + 1) * 512], True, False,
                   reads=[("aK", i), "QaT"], writes=[("ps", bs_)])
                n0 = kt // 2
                mm(PS(bs_), eselB[0:16, n0 * 128:(n0 + 1) * 128], MT[0:16, h, g * 512:(g + 1) * 512], False, True,
                   reads=["eselB"] + MT_res, writes=[("ps", bs_)])
                A("act", lambda e, bs_=bs_, kt=kt: e.activation(out=pT[kt % 2], in_=PS(bs_), func=AF.Exp, scale=SCALE),
                  reads=[("ps", bs_)], writes=[("pT", kt % 2)])
                for qs in range(4):
                    mm(psb[2 + qs][:, 0:129], pT[kt % 2][:, qs * 128:(qs + 1) * 128], Vh[i][:, kt, :], kt == 0, False,
                       reads=[("pT", kt % 2), ("aV", i)], writes=[("ps", 2 + qs)])
            for qs in range(4):
                Tq = 4 * g + qs
                tiles = [(Tq, True)] if Tq % 2 == 0 else [(Tq - 1, False), (Tq, True)]
                for ti, (ot, diag) in enumerate(tiles):
                    ei = ecnt[0] % 2
                    ecnt[0] += 1
                    mm(psb[6][:, 0:128], KaTo[:, h, ot * 128:(ot + 1) * 128], QaT[:, h, Tq * 128:(Tq + 1) * 128], True, True,
                       reads=["KaTo", "QaT"], writes=[("ps", 6)])
                    A("act", lambda e, ei=ei: e.activation(out=eo[ei], in_=psb[6][:, 0:128], func=AF.Exp, scale=SCALE),
                      reads=[("ps", 6)], writes=[("eo", ei)])
                    if diag:
                        A("dve", lambda e, ei=ei: e.tensor_tensor(out=eo[ei], in0=eo[ei], in1=triB, op=ALU.mult),
                          reads=[("eo", ei), "cstB"], writes=[("eo", ei)])
                    mm(psb[2 + qs][:, 0:129], eo[ei], Vao[:, ot, h, :], False, ti == len(tiles) - 1,
                       reads=[("eo", ei), "Vao"], writes=[("ps", 2 + qs)])
            finalize_heads(h, g, asb[h % 2], stg[h % 2], atTa_d, h % 2)
    P.barrier()
    if stop == 8:
        return nc, st, P

    def rs_from_ss(ss, rss):
        vv, rvv = smcol()
        rs, rrs = smcol()
        A("act", lambda e: e.activation(out=vv, in_=ss, func=AF.Ln, scale=1.0 / D, bias=epsc), reads=[rss, "neghalf"], writes=[rvv])
        A("act", lambda e: e.activation(out=rs, in_=vv, func=AF.Exp, scale=-0.5), reads=[rvv], writes=[rrs])
        return rs, rrs

    AR.reset()
    atA = AR.alloc([8, 512], BF16)
    atB = AR.alloc([8, 512], BF16)
    hTo = AR.alloc([16, 512], BF16)
    ycT = AR.alloc([16, 512], BF16)
    wso = WStream("wso", 2, [8, 512])
    wsg = WStream("wsg", 2, [16, 512])
    sg = [AR.alloc([512], F32) for _ in range(4)]
    ysb = AR.alloc([4, 2048], F32)
    GTB = AR.alloc([2048], F32)
    xt6 = [AR.alloc([2048], F32) for _ in range(2)]
    junk6 = AR.alloc([2048], BF16)
    dma("sp", GTB, gt_d[0].partition_broadcast(128), [], ["GTB"], "GTB")
    for g in range(2):
        gs = slice(g * 512, (g + 1) * 512)
        dma("sp", atA, atTa_d[:, :, gs].rearrange("h p t -> p h t"), [], ["atA"], "atA")
        dma("sp", atB, atTb_d[:, :, gs].rearrange("h p t -> p h t"), [], ["atB"], "atB")
        dma("sp", hTo, hTo_d[:, :, gs], [], ["hTo"], "hTo")
        for cc in range(4):
            wmo, rmo = wso.load(w_mo[:, cc * 512:(cc + 1) * 512].rearrange("(h p) n -> p h n", p=128))
            wdo, rdo = wso.load(w_do[:, cc * 512:(cc + 1) * 512].rearrange("(h p) n -> p h n", p=128))
            wga, rga = wsg.load(wsrc(w_in, C_GA + cc * 512, 512))
            wgb, rgb = wsg.load(wsrc(w_in, C_GB + cc * 512, 512))
            for ci in range(4):
                ct = cc * 4 + ci
                b0 = 4 * (ct % 2)
                cs_ = slice(ci * 128, (ci + 1) * 128)
                for h in range(8):
                    mm(PS(b0), wmo[:, h, cs_], atA[:, h, :], h == 0, h == 7, reads=[rmo, "atA"], writes=[("ps", b0)])
                for h in range(8):
                    mm(PS(b0 + 1), wdo[:, h, cs_], atB[:, h, :], h == 0, h == 7, reads=[rdo, "atB"], writes=[("ps", b0 + 1)])
                for kc in range(16):
                    mm(PS(b0 + 2), wga[:, kc, cs_], hTo[:, kc, :], kc == 0, kc == 15, reads=[rga, "hTo"], writes=[("ps", b0 + 2)])
                for kc in range(16):
                    mm(PS(b0 + 3), wgb[:, kc, cs_], hTo[:, kc, :], kc == 0, kc == 15, reads=[rgb, "hTo"], writes=[("ps", b0 + 3)])
                s0, s1 = sg[2 * (ct % 2)], sg[2 * (ct % 2) + 1]
                r0, r1 = ("sg", 2 * (ct % 2)), ("sg", 2 * (ct % 2) + 1)
                A("act", lambda e, b0=b0, s0=s0: e.activation(out=s0, in_=PS(b0 + 2), func=AF.Sigmoid), reads=[("ps", b0 + 2)], writes=[r0])
                A("act", lambda e, b0=b0, s1=s1: e.activation(out=s1, in_=PS(b0 + 3), func=AF.Sigmoid), reads=[("ps", b0 + 3)], writes=[r1])
                A("dve", lambda e, b0=b0, s0=s0: e.tensor_tensor(out=s0, in0=PS(b0), in1=s0, op=ALU.mult), reads=[("ps", b0), r0], writes=[r0])
                A("dve", lambda e, b0=b0, s1=s1: e.tensor_tensor(out=s1, in0=PS(b0 + 1), in1=s1, op=ALU.mult), reads=[("ps", b0 + 1), r1], writes=[r1])
                A("dve", lambda e, ct=ct, s0=s0, s1=s1: e.tensor_tensor(out=ycT[:, ct, :], in0=s0, in1=s1, op=ALU.add),
                  reads=[r0, r1], writes=[("ycT", ct)])
        yc_res = [("ycT", ct) for ct in range(16)]
        ycnt = [0]
        for cc in range(4):
            wo_, rwo = wsg.load(wsrc(w_o, cc * 512, 512))
            for tt in range(4):
                bank = ycnt[0] % 2
                ycnt[0] += 1
                for kc in range(16):
                    mm(PS(bank), ycT[:, kc, tt * 128:(tt + 1) * 128], wo_[:, kc, :], kc == 0, kc == 15,
                       reads=[rwo] + yc_res, writes=[("ps", bank)])
                A("act", lambda e, bank=bank, tt=tt, cc=cc: e.activation(out=ysb[:, tt, cc * 512:(cc + 1) * 512], in_=PS(bank), func=AF.Copy),
                  reads=[("ps", bank)], writes=[("ysb", tt)])
        for tt in range(4):
            row0 = g * 512 + tt * 128
            xt = xt6[tt % 2]
            rx = ("xt6", tt % 2)
            dma("sp", xt, xq[row0:row0 + 128, :], [], [rx], rx)
            ss, rss = smcol()
            A("act", lambda e, tt=tt, ss=ss: e.activation(out=junk6, in_=ysb[:, tt, :], func=AF.Square, accum_out=ss),
              reads=[("ysb", tt)], writes=["junk6", rss])
            rs, rrs = rs_from_ss(ss, rss)
            A("dve", lambda e, tt=tt, rs=rs: e.scalar_tensor_tensor(out=ysb[:, tt, :], in0=ysb[:, tt, :], scalar=rs, in1=GTB,
                                                                     op0=ALU.mult, op1=ALU.mult),
              reads=[("ysb", tt), rrs, "GTB"], writes=[("ysb", tt)])
            A("dve", lambda e, tt=tt, xt=xt: e.tensor_tensor(out=xt, in0=ysb[:, tt, :], in1=xt, op=ALU.add),
              reads=[("ysb", tt), rx], writes=[rx])
            dma("sp", x1_d[row0:row0 + 128, :], xt, [rx], [("x1d", row0)], ("x1st", tt % 2))
    P.barrier()
    if stop == 9:
        return nc, st, P

    for g in range(2):
        AR.reset()
        h2T = AR.alloc([16, 512], BF16)
        uT = AR.alloc([64, 512], BF16)
        mark = AR.off
        nctx = norm_setup()
        for tt in range(4):
            row0 = g * 512 + tt * 128
            norm_tile(nctx, x1_d[row0:row0 + 128, :], G2T, S2T,
                      (lambda kc, tt=tt: h2T[:, kc, tt * 128:(tt + 1) * 128]), (("h2T", tt, 0), ("h2T", tt, 1)))
        P.barrier()
        AR.off = mark
        wsf = WStream("wsf", 2, [16, 512])
        rsb = [AR.alloc([512], F32) for _ in range(2)]
        fsb = AR.alloc([4, 2048], F32)
        GT2B = AR.alloc([2048], F32)
        xt7 = [AR.alloc([2048], F32) for _ in range(2)]
        junk7 = AR.alloc([2048], BF16)
        dma("sp", GT2B, gt_d[1].partition_broadcast(128), [], ["GT2B"], "GT2B")
        for fc in range(16):
            w1, rw1 = wsf.load(wsrc(w_ff1, fc * 512, 512))
            for fi in range(4):
                ft = fc * 4 + fi
                bank = 2 + (ft % 2)
                for kc in range(16):
                    mm(PS(bank), w1[:, kc, fi * 128:(fi + 1) * 128], h2T[:, kc, :], kc == 0, kc == 15,
                       reads=[rw1, "h2Tall"], writes=[("ps", bank)])
                rr = rsb[ft % 2]
                A("act", lambda e, bank=bank, rr=rr: e.activation(out=rr, in_=PS(bank), func=AF.Relu),
                  reads=[("ps", bank)], writes=[("rsb", ft % 2)])
                A("dve", lambda e, bank=bank, rr=rr, ft=ft: e.scalar_tensor_tensor(
                    out=uT[:, ft, :], in0=PS(bank), scalar=0.0, in1=rr, op0=ALU.max, op1=ALU.mult),
                  reads=[("ps", bank), ("rsb", ft % 2)], writes=[("uT", ft)])
        for cc in range(4):
            for fq in range(4):
                w2, rw2 = wsf.load(w_ff2[fq * 2048:(fq + 1) * 2048, cc * 512:(cc + 1) * 512].rearrange("(ft p) n -> p ft n", p=128))
                for fi in range(16):
                    ft = fq * 16 + fi
                    for tt in range(4):
                        mm(PS(4 + tt), uT[:, ft, tt * 128:(tt + 1) * 128], w2[:, fi, :], ft == 0, ft == 63,
                           reads=[rw2, ("uT", ft)], writes=[("ps", 4 + tt)])
            for tt in range(4):
                A("act", lambda e, tt=tt, cc=cc: e.activation(out=fsb[:, tt, cc * 512:(cc + 1) * 512], in_=PS(4 + tt), func=AF.Copy),
                  reads=[("ps", 4 + tt)], writes=[("fsb", tt)])
        for tt in range(4):
            row0 = g * 512 + tt * 128
            xt = xt7[tt % 2]
            rx = ("xt7", tt % 2)
            dma("sp", xt, x1_d[row0:row0 + 128, :], [], [rx], rx)
            ss, rss = smcol()
            A("act", lambda e, tt=tt, ss=ss: e.activation(out=junk7, in_=fsb[:, tt, :], func=AF.Square, accum_out=ss),
              reads=[("fsb", tt)], writes=["junk7", rss])
            rs, rrs = rs_from_ss(ss, rss)
            A("dve", lambda e, tt=tt, rs=rs: e.scalar_tensor_tensor(out=fsb[:, tt, :], in0=fsb[:, tt, :], scalar=rs, in1=GT2B,
                                                                     op0=ALU.mult, op1=ALU.mult),
              reads=[("fsb", tt), rrs, "GT2B"], writes=[("fsb", tt)])
            A("dve", lambda e, tt=tt, xt=xt: e.tensor_tensor(out=xt, in0=fsb[:, tt, :], in1=xt, op=ALU.add),
              reads=[("fsb", tt), rx], writes=[rx])
            dma("sp", out_d[row0:row0 + 128, :], xt, [rx], [("outd", row0)], ("ost", tt % 2))
        P.barrier()
    return nc, st, P


def _rope_tables(pos):
    pos = np.asarray(pos, np.float32)
    out = np.zeros((4, 128, pos.shape[0]), np.float32)
    for which, half in ((0, 64), (2, 32)):
        inv = np.power(np.float32(10000.0), -np.arange(half, dtype=np.float32) / np.float32(half)).astype(np.float32)
        ang = (pos[None, :] * inv[:, None]).astype(np.float32)
        c = np.cos(ang).astype(np.float32)
        s = np.sin(ang).astype(np.float32)
        d = np.arange(128)
        dd = d % (2 * half)
        idx = dd % half
        sign = np.where(dd < half, -1.0, 1.0).astype(np.float32)
        out[which] = c[idx]
        out[which + 1] = s[idx] * sign[:, None]
    return out


def _consts(qpos):
    c = np.zeros((128, 5 * 128 + 8), np.float32)
    c[:, 0:128] = np.eye(128, dtype=np.float32)
    m = np.arange(128)
    p128 = np.zeros((128, 128), np.float32)
    p128[(m + 64) % 128, m] = 1.0
    c[:, 128:256] = p128
    p64 = np.zeros((128, 128), np.float32)
    p64[(m // 64) * 64 + ((m % 64) + 32) % 64, m] = 1.0
    c[:, 256:384] = p64
    k = np.arange(128)[:, None]
    q = np.arange(128)[None, :]
    c[:, 384:512] = (k <= q).astype(np.float32)
    n = np.arange(128) % 16
    c[:, 512:640] = ((n + 1) * 256).astype(np.float32)[None, :]
    c[:, 640:648] = qpos.reshape(8, 128).T
    return c


def prep_inputs(core, x, c, w_ada, b_ada, g_pre_mix, g_post_mix, w_in, w_moba_out, w_dsa_out, w_o,
                g_pre_ffn, g_post_ffn, w_ff1, w_ff2, shared=None):
    b, j = core // 4, core % 4
    blkA, blkB = j, 7 - j
    own = np.concatenate([np.arange(blkA * 512, blkA * 512 + 512), np.arange(blkB * 512, blkB * 512 + 512)])
    qpos = own.astype(np.float32)
    f = lambda a: np.ascontiguousarray(a, dtype=np.float32)
    tcol = lambda v, n: f(np.asarray(v).reshape(n, 128).T)
    if shared is None:
        shared = {}
    if "w_ada" not in shared:
        esel = np.zeros((16, 16, 128), np.float32)
        for n0 in range(16):
            esel[n0, n0, :] = 1.0
        shared.update(
            w_ada=f(w_ada[0]), b_adaT=tcol(b_ada[0], 96),
            gT=f(np.concatenate([tcol(g_pre_mix[0], 16), tcol(g_post_mix[0], 16),
                                 tcol(g_pre_ffn[0], 16), tcol(g_post_ffn[0], 16)], axis=1)),
            w_in=f(w_in[0]), w_ki2=f(np.concatenate([w_in[0][:, C_KI:C_KI + 64], w_in[0][:, C_KI:C_KI + 64]], axis=1)),
            w_mo=f(w_moba_out[0]), w_do=f(w_dsa_out[0]), w_o=f(w_o[0]), w_ff1=f(w_ff1[0]), w_ff2=f(w_ff2[0]),
            ropeF=_rope_tables(np.arange(T, dtype=np.float32)),
            esel=f(esel.reshape(16, 16 * 128)),
            kposB=f(np.broadcast_to(np.arange(T, dtype=np.float32)[None, :], (128, T))),
        )
    m = dict(shared)
    m.update(
        xfull=f(x[b]), xq=f(x[b][own]), cT=tcol(c[b], 16),
        ropeQ=_rope_tables(qpos), cst=_consts(qpos),
    )
    return m, b, own


_CACHE = {}


def kernel(**inputs):
    inputs = {k: np.asarray(v) for k, v in inputs.items()}
    if "nc" not in _CACHE:
        nc, st, P = build()
        P.emit(st)
        st.close()
        _CACHE["nc"] = nc
    nc = _CACHE["nc"]
    shared = {}
    in_maps, owns = [], []
    for core in range(8):
        m, b, own = prep_inputs(core, shared=shared, **inputs)
        in_maps.append(m)
        owns.append((b, own))
    res = run_bass_kernel_spmd(nc, in_maps, core_ids=list(range(8)))
    out = np.empty((2, T, D), np.float32)
    for core in range(8):
        b, own = owns[core]
        out[b, own] = np.asarray(res.results[core]["out"])
    return out
```

```python
import numpy as np
from contextlib import ExitStack
import concourse.bass as bass
import concourse.mybir as mybir
from concourse.bass_utils import run_bass_kernel_spmd

F32 = mybir.dt.float32
BF16 = mybir.dt.bfloat16
U32 = mybir.dt.uint32
ALU = mybir.AluOpType
AF = mybir.ActivationFunctionType
AX = mybir.AxisListType

ENGS = ("pe", "act", "dve", "pool", "sp")


class Op:
    __slots__ = ("eng", "fn", "deps", "sig", "cnt", "sem", "is_dma", "idx", "nosync_same")

    def __init__(self, eng, fn, is_dma):
        self.eng = eng
        self.fn = fn
        self.is_dma = is_dma
        self.deps = []
        self.sig = False
        self.cnt = 0
        self.sem = None
        self.idx = -1
        self.nosync_same = False


class _Rec:
    def __getattr__(self, name):
        def f(*a, **k):
            self.call = (name, a, k)
            return self
        return f


class Prog:
    def __init__(self, nc):
        self.nc = nc
        self.ops = {e: [] for e in ENGS}
        self.last_w = {}
        self.readers = {}
        self.dma_keys = {}
        self.n_ops = 0

    def _collect(self, op, reads, writes):
        deps = {}

        def add_dep(d):
            if d is None or d is op:
                return
            if d.is_dma:
                deps[("dma", id(d))] = d
            else:
                k = ("eng", d.eng)
                if k not in deps or deps[k].idx < d.idx:
                    deps[k] = d

        for r in reads:
            add_dep(self.last_w.get(r))
        for r in writes:
            add_dep(self.last_w.get(r))
            rd = self.readers.get(r)
            if rd:
                for d in rd.values():
                    add_dep(d)
        return deps

    def add(self, eng, fn, reads=(), writes=(), dma_key=None, pe_acc=False):
        is_dma = dma_key is not None
        rec = _Rec()
        fn(rec)
        _name, _a, _k = rec.call
        fn = (lambda eng, _name=_name, _a=_a, _k=_k: getattr(eng, _name)(*_a, **_k))
        writes = list(writes) + [r for r in reads if isinstance(r, tuple) and r[0] == "ps" and r not in writes]
        op = Op(eng, fn, is_dma)
        op.idx = len(self.ops[eng])
        op.nosync_same = pe_acc
        deps = self._collect(op, reads, writes)
        if is_dma:
            ent = self.dma_keys.setdefault(dma_key, [None, None, 0])
            if ent[1] is not None:
                deps[("dma", id(ent[1]))] = ent[1]
            ent[2] += 16
            op.cnt = ent[2]
            op.sem = dma_key
            ent[1] = op
        dl = []
        for d in deps.values():
            if (not d.is_dma) and d.eng == eng and (pe_acc or eng in ("sp", "pe")):
                continue
            dl.append(d)
            d.sig = True
        op.deps = dl
        self.ops[eng].append(op)
        for r in reads:
            rd = self.readers.setdefault(r, {})
            rd[("dma", id(op)) if is_dma else ("eng", eng)] = op
        for r in writes:
            self.last_w[r] = op
            self.readers[r] = {}
        self.n_ops += 1
        return op

    def emit(self, stack, final_waits=()):
        nc = self.nc
        esem = {e: stack.enter_context(nc.semaphore("s_" + e)) for e in ENGS}
        for i, (k, ent) in enumerate(self.dma_keys.items()):
            ent[0] = stack.enter_context(nc.semaphore("d%d" % i))
        for e in ENGS:
            c = 0
            for op in self.ops[e]:
                if op.is_dma:
                    continue
                if op.sig:
                    c += 1
                    op.cnt = c
        block = stack.enter_context(nc.Block())

        def run(e, eng):
            known = {}
            for op in self.ops[e]:
                for d in op.deps:
                    if d.is_dma:
                        s = self.dma_keys[d.sem][0]
                        key = ("d", d.sem)
                    else:
                        s = esem[d.eng]
                        key = ("e", d.eng)
                    if known.get(key, 0) >= d.cnt:
                        continue
                    eng.wait_ge(s, d.cnt)
                    known[key] = d.cnt
                if op.fn is None:
                    continue
                ins = op.fn(eng)
                if op.is_dma:
                    ins.then_inc(self.dma_keys[op.sem][0], 16)
                elif op.sig:
                    ins.then_inc(esem[e], 1)
            if e == "sp":
                for k in final_waits:
                    ent = self.dma_keys[k]
                    eng.wait_ge(ent[0], ent[2])

        @block.tensor
        def _(eng):
            run("pe", eng)

        @block.scalar
        def _(eng):
            run("act", eng)

        @block.vector
        def _(eng):
            run("dve", eng)

        @block.gpsimd
        def _(eng):
            run("pool", eng)

        @block.sync
        def _(eng):
            run("sp", eng)

    def barrier(self):
        lasts = []
        for e in ENGS:
            for op in reversed(self.ops[e]):
                if not op.is_dma and op.fn is not None:
                    lasts.append(op)
                    break
        for ent in self.dma_keys.values():
            if ent[1] is not None:
                lasts.append(ent[1])
        for e in ENGS:
            op = Op(e, None, False)
            op.idx = len(self.ops[e])
            dl = []
            for d in lasts:
                if (not d.is_dma) and d.eng == e and e in ("pe", "sp"):
                    continue
                d.sig = True
                dl.append(d)
            op.deps = dl
            self.ops[e].append(op)
        self.last_w = {}
        self.readers = {}


D = 2048
T = 4096
NOWN = 1024
DIN = 11344
DFF = 8192
BIGM = 30000.0
SCALE = 128.0 ** -0.5
C_QA, C_KA, C_VA, C_QB, C_KB, C_VB, C_QI, C_KI, C_WI, C_GA, C_GB = (
    0, 1024, 2048, 3072, 4096, 5120, 6144, 7168, 7232, 7248, 9296)
ARENA_BYTES = 176 * 1024
N_BISECT = 22


class Arena:
    def __init__(self, t, nbytes):
        self.t = t
        self.n = nbytes
        self.off = 0

    def reset(self):
        self.off = 0

    def alloc(self, shape, dt):
        n = 1
        for s in shape:
            n *= s
        esz = 4 if dt == F32 else 2
        nb = (n * esz + 63) // 64 * 64
        o = self.off
        self.off += nb
        assert self.off <= self.n, ("arena overflow", self.off, self.n)
        ap = self.t[:, o // 2:(o + n * esz) // 2]
        if dt == F32:
            ap = ap.bitcast(F32)
        if len(shape) == 2:
            ap = ap.rearrange("p (a b) -> p a b", a=shape[0])
        elif len(shape) == 3:
            ap = ap.rearrange("p (a b c) -> p a b c", a=shape[0], b=shape[1])
        return ap


def build(debug=(), stop=None):
    nc = bass.Bass("TRN2", target_bir_lowering=False)
    st = ExitStack()

    def din(name, shape, dt=F32):
        return nc.dram_tensor(name, list(shape), dt, kind="ExternalInput").ap()

    def dscr(name, shape, dt):
        kind = "ExternalOutput" if name in debug else "Internal"
        return nc.dram_tensor(name, list(shape), dt, kind=kind).ap()

    xfull = din("xfull", [T, D])
    xq = din("xq", [NOWN, D])
    cT_d = din("cT", [128, 16])
    w_ada = din("w_ada", [D, 6 * D])
    b_adaT_d = din("b_adaT", [128, 96])
    gT_d = din("gT", [128, 64])
    w_in = din("w_in", [D, DIN])
    w_ki2 = din("w_ki2", [D, 128])
    w_mo = din("w_mo", [1024, D])
    w_do = din("w_do", [1024, D])
    w_o = din("w_o", [D, D])
    w_ff1 = din("w_ff1", [D, DFF])
    w_ff2 = din("w_ff2", [DFF, D])
    ropeF = din("ropeF", [4, 128, T])
    ropeQ = din("ropeQ", [4, 128, NOWN])
    cst = din("cst", [128, 5 * 128 + 8 + 32])
    esel_d = din("esel", [128, 16 * 128])
    kpos_d = din("kposB", [128, T])
    out_d = nc.dram_tensor("out", [NOWN, D], F32, kind="ExternalOutput").ap()

    kTa_d = dscr("kTa", [8, 128, T], BF16)
    kTb_d = dscr("kTb", [8, 128, T], BF16)
    va_d = dscr("va", [8, 128, 32, 129], BF16)
    vb_d = dscr("vb", [8, 128, 32, 129], BF16)
    QaT_d = dscr("QaT", [8, 128, NOWN], BF16)
    QbT_d = dscr("QbT", [8, 128, NOWN], BF16)
    QiT_d = dscr("QiT", [8, 128, NOWN], BF16)
    KaTo_d = dscr("KaTo", [8, 128, NOWN], BF16)
    Vao_d = dscr("Vao", [128, 8, 8, 129], BF16)
    hTo_d = dscr("hTo", [128, 16, NOWN], BF16)
    atTa_d = dscr("atTa", [8, 128, NOWN], BF16)
    atTb_d = dscr("atTb", [8, 128, NOWN], BF16)
    gt_d = dscr("gtrow", [2, D], F32)
    x1_d = dscr("x1", [NOWN, D], F32)
    dbg_d = dscr("dbg", [128, 4096], F32)

    arena_t = st.enter_context(nc.sbuf_tensor("sb_arena", [128, ARENA_BYTES // 2], BF16))
    AR = Arena(arena_t, ARENA_BYTES)
    cstF = st.enter_context(nc.sbuf_tensor("sb_cstF", [128, 5 * 128 + 8 + 32], F32))
    cstB = st.enter_context(nc.sbuf_tensor("sb_cstB", [128, 4 * 128], BF16))
    eselB = st.enter_context(nc.sbuf_tensor("sb_eselB", [128, 16 * 128], BF16))
    modT = st.enter_context(nc.sbuf_tensor("sb_modT", [128, 96], F32))
    gT = st.enter_context(nc.sbuf_tensor("sb_gT", [128, 64], F32))
    vecs = st.enter_context(nc.sbuf_tensor("sb_vecs", [128, 6 * 16], F32))
    kiT2 = st.enter_context(nc.sbuf_tensor("sb_kiT2", [128, T], BF16))
    ksum = st.enter_context(nc.sbuf_tensor("sb_ksum", [128, 128], F32))
    kmeanT = st.enter_context(nc.sbuf_tensor("sb_kmeanT", [128, 128], BF16))
    wabs = st.enter_context(nc.sbuf_tensor("sb_wabs", [128, 128], F32))
    wsgn = st.enter_context(nc.sbuf_tensor("sb_wsgn", [128, 128], F32))
    small = st.enter_context(nc.sbuf_tensor("sb_small", [128, 64], F32))
    psb = [st.enter_context(nc.psum_tensor("ps%d" % i, [128, 512], F32)) for i in range(8)]

    identF = cstF[:, 0:128]
    blockend = cstF[:, 512:640]
    qposT = cstF[:, 640:648]
    ftab = cstF[:, 648:680]
    identB = cstB[:, 0:128]
    psw128 = cstB[:, 128:256]
    psw64 = cstB[:, 256:384]
    triB = cstB[:, 384:512]
    G1T, S1T, GT1T = vecs[:, 0:16], vecs[:, 16:32], vecs[:, 32:48]
    G2T, S2T, GT2T = vecs[:, 48:64], vecs[:, 64:80], vecs[:, 80:96]
    neghalf = small[:, 0:1]
    epsc = small[:, 1:2]
    sm_ctr = [2]

    def smcol(n=1):
        if sm_ctr[0] + n > 64:
            sm_ctr[0] = 2
        a = sm_ctr[0]
        sm_ctr[0] += n
        return small[:, a:a + n], ("small", a)

    P = Prog(nc)
    A = P.add

    def PS(i):
        return psb[i][:, :]

    def PSB(i):
        return psb[i][:, :].bitcast(BF16)

    def dma(eng, out, in_, reads, writes, key, nonc=False):
        if nonc:
            return A(eng, lambda e: e.dma_start(out=out, in_=in_, allow_slow_non_contiguous=True),
                     reads=reads, writes=writes, dma_key=key)
        return A(eng, lambda e: e.dma_start(out=out, in_=in_), reads=reads, writes=writes, dma_key=key)

    def mm(out, lhsT, rhs, start, stop, reads, writes):
        return A("pe", lambda e: e.matmul(out, lhsT, rhs, start=start, stop=stop), reads=reads, writes=writes)

    def tr(out, in_, ident, reads, writes):
        return A("pe", lambda e: e.transpose(out, in_, ident), reads=reads, writes=writes)

    dma("sp", cstF[:, :], cst, [], ["cstF"], "c0")
    dma("sp", gT[:, :], gT_d, [], ["gT"], "c2")
    A("dve", lambda e: e.tensor_copy(out=cstB[:, :], in_=cstF[:, 0:512]), reads=["cstF"], writes=["cstB"])
    A("dve", lambda e: e.memset(neghalf, -0.5), writes=["neghalf"])
    A("dve", lambda e: e.memset(epsc, 1e-6), reads=["neghalf"], writes=["neghalf"])
    A("dve", lambda e: e.memset(ksum[:, :], 0.0), writes=["ksum"])

    AR.reset()
    cT = AR.alloc([16], F32)
    csT = AR.alloc([16], BF16)
    badaT = AR.alloc([96], F32)
    eselF = AR.alloc([16 * 128], F32)
    wada = [AR.alloc([16, 512], BF16) for _ in range(3)]
    dma("sp", cT, cT_d, [], ["cT"], "c3")
    dma("sp", badaT, b_adaT_d, [], ["badaT"], "c4")
    dma("sp", eselF, esel_d, [], ["eselF"], "c1")
    A("dve", lambda e: e.tensor_copy(out=eselB[:, :], in_=eselF), reads=["eselF"], writes=["eselB"])
    A("act", lambda e: e.activation(out=csT, in_=cT, func=AF.Silu), reads=["cT"], writes=["csT"])

    def load_wada(c):
        src = w_ada[:, c * 512:(c + 1) * 512].rearrange("(kc p) n -> p kc n", p=128)
        dma("pool", wada[c % 3], src, [], [("wada", c % 3)], ("wada", c % 3))

    load_wada(0)
    load_wada(1)
    for c in range(24):
        if c + 2 < 24:
            load_wada(c + 2)
        wb = wada[c % 3]
        for ci in range(4):
            ct = c * 4 + ci
            for kc in range(16):
                mm(psb[0][:, ct:ct + 1], wb[:, kc, ci * 128:(ci + 1) * 128], csT[:, kc:kc + 1],
                   kc == 0, kc == 15, reads=[("wada", c % 3), "csT"], writes=[("ps", 0)])
    A("dve", lambda e: e.tensor_tensor(out=modT[:, :], in0=psb[0][:, 0:96], in1=badaT, op=ALU.add),
      reads=[("ps", 0), "badaT"], writes=["modT"])
    A("dve", lambda e: e.scalar_tensor_tensor(out=G1T, in0=modT[:, 16:32], scalar=1.0, in1=gT[:, 0:16],
                                               op0=ALU.add, op1=ALU.mult), reads=["modT", "gT"], writes=["vecs"])
    A("dve", lambda e: e.tensor_copy(out=S1T, in_=modT[:, 0:16]), reads=["modT", "vecs"], writes=["vecs"])
    A("dve", lambda e: e.tensor_tensor(out=GT1T, in0=modT[:, 32:48], in1=gT[:, 16:32], op=ALU.mult),
      reads=["modT", "gT", "vecs"], writes=["vecs"])
    A("dve", lambda e: e.scalar_tensor_tensor(out=G2T, in0=modT[:, 64:80], scalar=1.0, in1=gT[:, 32:48],
                                               op0=ALU.add, op1=ALU.mult), reads=["modT", "gT", "vecs"], writes=["vecs"])
    A("dve", lambda e: e.tensor_copy(out=S2T, in_=modT[:, 48:64]), reads=["modT", "vecs"], writes=["vecs"])
    A("dve", lambda e: e.tensor_tensor(out=GT2T, in0=modT[:, 80:96], in1=gT[:, 48:64], op=ALU.mult),
      reads=["modT", "gT", "vecs"], writes=["vecs"])
    dma("sp", gt_d[0].rearrange("(kc p) -> p kc", p=128), GT1T, ["vecs"], ["gt_d"], "c5", nonc=True)
    dma("sp", gt_d[1].rearrange("(kc p) -> p kc", p=128), GT2T, ["vecs"], ["gt_d"], "c5", nonc=True)
    P.barrier()
    if stop == 0:
        return nc, st, P

    class NormCtx:
        pass

    def norm_setup():
        n = NormCtx()
        n.xt = [AR.alloc([D], F32) for _ in range(2)]
        n.xs = [AR.alloc([D], BF16) for _ in range(2)]
        n.junk = AR.alloc([D], BF16)
        n.i = 0
        return n

    def norm_tile(n, src_rows, G, S, dst_fn, dst_res2, pbanks=(0, 1)):
        i = n.i
        n.i += 1
        xt, xs = n.xt[i % 2], n.xs[i % 2]
        rxt, rxs = ("xt", i % 2), ("xs", i % 2)
        dma("sp", xt, src_rows, [], [rxt], rxt)
        ss, rss = smcol()
        vv, rvv = smcol()
        rs, rrs = smcol()
        A("act", lambda e: e.activation(out=n.junk, in_=xt, func=AF.Square, accum_out=ss),
          reads=[rxt], writes=["njunk", rss])
        import os
        KN = int(os.environ.get("KN", "9"))
        if KN < 1:
            return
        A("act", lambda e: e.activation(out=vv, in_=ss, func=AF.Ln, scale=1.0 / D, bias=epsc),
          reads=[rss, "neghalf"], writes=[rvv])
        A("act", lambda e: e.activation(out=rs, in_=vv, func=AF.Exp, scale=-0.5), reads=[rvv], writes=[rrs])
        if KN < 2:
            return
        A("act", lambda e: e.activation(out=xs, in_=xt, func=AF.Copy, scale=rs), reads=[rxt, rrs], writes=[rxs])
        if KN < 3:
            return
        for kc in range(16):
            b = pbanks[kc // 8]
            tr(PSB(b)[:, (kc % 8) * 128:(kc % 8 + 1) * 128], xs[:, kc * 128:(kc + 1) * 128], identB,
               reads=[rxs, "cstB"], writes=[("ps", b)])
        if KN < 4:
            return
        for kc in range(16):
            b = pbanks[kc // 8]
            src = PSB(b)[:, (kc % 8) * 128:(kc % 8 + 1) * 128]
            dst = dst_fn(kc)
            KE = os.environ.get("KE", "mix")
            if (kc // 8 == 0 and KE == "mix") or KE == "dve":
                A("dve", lambda e, src=src, dst=dst, kc=kc: e.tensor_scalar(
                    out=dst, in0=src, scalar1=G[:, kc:kc + 1], scalar2=S[:, kc:kc + 1], op0=ALU.mult, op1=ALU.add),
                  reads=[("ps", b), "vecs"], writes=[dst_res2[0]])
            else:
                A("act", lambda e, src=src, dst=dst, kc=kc: e.activation(
                    out=dst, in_=src, func=AF.Identity, scale=G[:, kc:kc + 1], bias=S[:, kc:kc + 1]),
                  reads=[("ps", b), "vecs"], writes=[dst_res2[1]])

    class WStream:
        def __init__(self, name, nbuf, shape):
            self.name = name
            self.bufs = [AR.alloc(shape, BF16) for _ in range(nbuf)]
            self.n = 0

        def load(self, src_ap, view=None):
            i = self.n % len(self.bufs)
            self.n += 1
            dst = self.bufs[i] if view is None else view(self.bufs[i])
            r = (self.name, i)
            dma("pool", dst, src_ap, [], [r], r)
            return self.bufs[i], r

    def wsrc(w, c0, ncols):
        return w[:, c0:c0 + ncols].rearrange("(kc p) n -> p kc n", p=128)

    class RopeCtx:
        pass

    def rope_setup():
        r = RopeCtx()
        r.xb = [AR.alloc([512], BF16) for _ in range(2)]
        r.t1 = [AR.alloc([512], F32) for _ in range(2)]
        r.t2 = [AR.alloc([512], F32) for _ in range(2)]
        r.i = 0
        return r

    def rope(r, pbank, swbank, cosT, sinT, psw, out_ap, out_res, tab_res):
        i = r.i
        r.i += 1
        xb, t1, t2 = r.xb[i % 2], r.t1[i % 2], r.t2[i % 2]
        import os
        KR = int(os.environ.get("KR", "9"))
        if KR < 1:
            return
        A("act", lambda e: e.activation(out=xb, in_=PS(pbank), func=AF.Copy), reads=[("ps", pbank)], writes=[("rxb", i % 2)])
        if KR < 2:
            return
        mm(PS(swbank), psw, xb, True, True, reads=[("rxb", i % 2), "cstB"], writes=[("ps", swbank)])
        if KR < 3:
            return
        A("dve", lambda e: e.tensor_tensor(out=t1, in0=PS(pbank), in1=cosT, op=ALU.mult),
          reads=[("ps", pbank), tab_res], writes=[("rt1", i % 2)])
        A("dve", lambda e: e.tensor_tensor(out=t2, in0=PS(swbank), in1=sinT, op=ALU.mult),
          reads=[("ps", swbank), tab_res], writes=[("rt2", i % 2)])
        if KR < 4:
            return
        A("dve", lambda e: e.tensor_tensor(out=out_ap, in0=t1, in1=t2, op=ALU.add),
          reads=[("rt1", i % 2), ("rt2", i % 2)], writes=[out_res])

    AR.reset()
    nctx = norm_setup()
    rctx = rope_setup()
    hT = AR.alloc([16, 1024], BF16)
    ws = WStream("w1", 3, [16, 512])
    tabs = AR.alloc([4, 1024], F32)
    kst = [AR.alloc([1024], BF16) for _ in range(2)]
    vst = [AR.alloc([4, 129], BF16) for _ in range(3)]
    for i in range(3):
        A("dve", lambda e, i=i: e.memset(vst[i][:, :, 128:129], 1.0), writes=[("vst", i)])
    kcnt = [0]
    vcnt = [0]
    pk = [0]
    for tg in range(4):
        t0 = tg * 1024
        dma("sp", tabs, ropeF[:, :, t0:t0 + 1024].rearrange("f p t -> p f t"), [], ["tabs"], "tabs")
        for tt in range(8):
            norm_tile(nctx, xfull[t0 + tt * 128:t0 + (tt + 1) * 128, :], G1T, S1T,
                      (lambda kc, tt=tt: hT[:, kc, tt * 128:(tt + 1) * 128]), (("hT", tt, 0), ("hT", tt, 1)))
        hT_res = [("hT", tt, q) for tt in range(8) for q in range(2)]
        if stop == 1:
            P.barrier()
            return nc, st, P
        kjobs = []
        kjobs.append((wsrc(w_in, C_KA, 512), 512, [(ci, "a", ci) for ci in range(4)]))
        kjobs.append((wsrc(w_in, C_KA + 512, 512), 512, [(ci, "a", 4 + ci) for ci in range(4)]))
        kjobs.append((wsrc(w_in, C_KB, 512), 512, [(ci, "b", ci) for ci in range(4)]))
        kjobs.append((wsrc(w_in, C_KB + 512, 512), 512, [(ci, "b", 4 + ci) for ci in range(4)]))
        kjobs.append((wsrc(w_ki2, 0, 128), 128, [(0, "i", 0)]))
        for (src, ncols, tiles) in kjobs:
            wb, wr = ws.load(src, view=(lambda b, ncols=ncols: b[:, :, 0:ncols]))
            for (ci, kind, head) in tiles:
                ks = kst[kcnt[0] % 2]
                ksr = ("kst", kcnt[0] % 2)
                kcnt[0] += 1
                for half in range(2):
                    pb = 2 + (pk[0] % 2)
                    sb_ = 4 + (pk[0] % 2)
                    pk[0] += 1
                    for kc in range(16):
                        mm(PS(pb), wb[:, kc, ci * 128:(ci + 1) * 128], hT[:, kc, half * 512:(half + 1) * 512],
                           kc == 0, kc == 15, reads=[wr] + hT_res[half * 8:half * 8 + 8], writes=[("ps", pb)])
                    if kind == "i":
                        cosT, sinT, psw = tabs[:, 2, half * 512:(half + 1) * 512], tabs[:, 3, half * 512:(half + 1) * 512], psw64
                        oap, ores = kiT2[:, t0 + half * 512:t0 + (half + 1) * 512], "kiT2"
                    else:
                        cosT, sinT, psw = tabs[:, 0, half * 512:(half + 1) * 512], tabs[:, 1, half * 512:(half + 1) * 512], psw128
                        oap, ores = ks[:, half * 512:(half + 1) * 512], ksr
                    rope(rctx, pb, sb_, cosT, sinT, psw, oap, ores, "tabs")
                if kind == "a":
                    A("dve", lambda e, ks=ks, head=head, tg=tg: e.tensor_reduce(
                        out=ksum[:, head * 16 + tg * 4:head * 16 + tg * 4 + 4],
                        in_=ks.rearrange("p (n j) -> p n j", j=256), axis=AX.X, op=ALU.add),
                      reads=[ksr], writes=["ksum"])
                if kind in ("a", "b"):
                    dst = (kTa_d if kind == "a" else kTb_d)[head, :, t0:t0 + 1024]
                    dma("sp", dst, ks, [ksr], [("kT", kind, head)], ("kstore", kcnt[0] % 2))
        if stop == 2:
            P.barrier()
            return nc, st, P
        for (c0, vd, hb) in ((C_VA, va_d, 0), (C_VA + 512, va_d, 4), (C_VB, vb_d, 0), (C_VB + 512, vb_d, 4)):
            wb, wr = ws.load(wsrc(w_in, c0, 512))
            for tt in range(8):
                pb = 6 + (vcnt[0] % 2)
                vs = vst[vcnt[0] % 3]
                vsr = ("vst", vcnt[0] % 3)
                vcnt[0] += 1
                for kc in range(16):
                    mm(PS(pb), hT[:, kc, tt * 128:(tt + 1) * 128], wb[:, kc, :], kc == 0, kc == 15,
                       reads=[wr, ("hT", tt, 0), ("hT", tt, 1)], writes=[("ps", pb)])
                A("act", lambda e, vs=vs, pb=pb: e.activation(
                    out=vs[:, :, 0:128], in_=PS(pb).rearrange("p (h d) -> p h d", h=4), func=AF.Copy),
                  reads=[("ps", pb)], writes=[vsr])
                gtile = tg * 8 + tt
                dma("sp", vd[hb:hb + 4, :, gtile, :].rearrange("h p c -> p h c"), vs, [vsr],
                    [("vd", id(vd), hb, gtile)], ("vstore", vcnt[0] % 3))
        if stop == 3:
            P.barrier()
            return nc, st, P
    A("dve", lambda e: e.tensor_scalar(out=kmeanT[:, :], in0=ksum[:, :], scalar1=1.0 / 256.0, scalar2=None, op0=ALU.mult),
      reads=["ksum"], writes=["kmeanT"])
    P.barrier()
    if stop == 4:
        return nc, st, P

    AR.reset()
    nctx = norm_setup()
    rctx = rope_setup()
    hT = AR.alloc([16, 1024], BF16)
    ws = WStream("w2", 3, [16, 512])
    tabs = AR.alloc([4, 1024], F32)
    kst = [AR.alloc([1024], BF16) for _ in range(2)]
    vst = [AR.alloc([4, 129], BF16) for _ in range(3)]
    wwi = AR.alloc([16, 16], BF16)
    for i in range(3):
        A("dve", lambda e, i=i: e.memset(vst[i][:, :, 128:129], 1.0), writes=[("vst", i)])
    dma("sp", tabs, ropeQ.rearrange("f p t -> p f t"), [], ["tabs"], "tabs")
    dma("pool", wwi, w_in[:, C_WI:C_WI + 16].rearrange("(kc p) n -> p kc n", p=128), [], ["wwi"], "wwi")
    for tt in range(8):
        norm_tile(nctx, xq[tt * 128:(tt + 1) * 128, :], G1T, S1T,
                  (lambda kc, tt=tt: hT[:, kc, tt * 128:(tt + 1) * 128]), (("hT", tt, 0), ("hT", tt, 1)))
    hT_res = [("hT", tt, q) for tt in range(8) for q in range(2)]
    dma("sp", hTo_d, hT, hT_res, ["hTo_d"], "hTo")
    kcnt = [0]
    pk = [0]
    for (c0, dst_d, r64) in ((C_QA, QaT_d, False), (C_KA, KaTo_d, False), (C_QB, QbT_d, False), (C_QI, QiT_d, True)):
        for ch in range(2):
            wb, wr = ws.load(wsrc(w_in, c0 + ch * 512, 512))
            for ci in range(4):
                head = ch * 4 + ci
                ks = kst[kcnt[0] % 2]
                ksr = ("kst", kcnt[0] % 2)
                kcnt[0] += 1
                for half in range(2):
                    pb = 2 + (pk[0] % 2)
                    sb_ = 4 + (pk[0] % 2)
                    pk[0] += 1
                    for kc in range(16):
                        mm(PS(pb), wb[:, kc, ci * 128:(ci + 1) * 128], hT[:, kc, half * 512:(half + 1) * 512],
                           kc == 0, kc == 15, reads=[wr] + hT_res[half * 8:half * 8 + 8], writes=[("ps", pb)])
                    sl = slice(half * 512, (half + 1) * 512)
                    if r64:
                        rope(rctx, pb, sb_, tabs[:, 2, sl], tabs[:, 3, sl], psw64, ks[:, sl], ksr, "tabs")
                    else:
                        rope(rctx, pb, sb_, tabs[:, 0, sl], tabs[:, 1, sl], psw128, ks[:, sl], ksr, "tabs")
                dma("sp", dst_d[head, :, :], ks, [ksr], [("qd", c0, head)], ("kstore", kcnt[0] % 2))
    vcnt = [0]
    for ch in range(2):
        wb, wr = ws.load(wsrc(w_in, C_VA + ch * 512, 512))
        for tt in range(8):
            pb = 6 + (vcnt[0] % 2)
            vs = vst[vcnt[0] % 3]
            vsr = ("vst", vcnt[0] % 3)
            vcnt[0] += 1
            for kc in range(16):
                mm(PS(pb), hT[:, kc, tt * 128:(tt + 1) * 128], wb[:, kc, :], kc == 0, kc == 15,
                   reads=[wr, ("hT", tt, 0), ("hT", tt, 1)], writes=[("ps", pb)])
            A("act", lambda e, vs=vs, pb=pb: e.activation(
                out=vs[:, :, 0:128], in_=PS(pb).rearrange("p (h d) -> p h d", h=4), func=AF.Copy),
              reads=[("ps", pb)], writes=[vsr])
            dma("sp", Vao_d[:, tt, ch * 4:ch * 4 + 4, :], vs, [vsr], [("vao", tt, ch)], ("vstore", vcnt[0] % 3))
    for tt in range(8):
        pb = 6 + (tt % 2)
        for kc in range(16):
            mm(psb[pb][:, 0:16], hT[:, kc, tt * 128:(tt + 1) * 128], wwi[:, kc, :], kc == 0, kc == 15,
               reads=["wwi", ("hT", tt, 0), ("hT", tt, 1)], writes=[("ps", pb)])
        A("act", lambda e, tt=tt, pb=pb: e.activation(out=wabs[:, tt * 16:(tt + 1) * 16], in_=psb[pb][:, 0:16], func=AF.Abs),
          reads=[("ps", pb)], writes=["wabs"])
        A("act", lambda e, tt=tt, pb=pb: e.activation(out=wsgn[:, tt * 16:(tt + 1) * 16], in_=psb[pb][:, 0:16], func=AF.Sign),
          reads=[("ps", pb)], writes=["wsgn"])
    P.barrier()
    if stop == 5:
        return nc, st, P

    def finalize_heads(h, g, asb, stg, dst_d, ia):
        for qs in range(4):
            rd, rrd = smcol()
            A("dve", lambda e, rd=rd, qs=qs: e.reciprocal(out=rd, in_=psb[2 + qs][:, 128:129]),
              reads=[("ps", 2 + qs)], writes=[rrd])
            A("act", lambda e, rd=rd, qs=qs: e.activation(out=asb[:, qs, :], in_=psb[2 + qs][:, 0:128], func=AF.Copy, scale=rd),
              reads=[("ps", 2 + qs), rrd], writes=[("asb", ia, qs)])
        for qs in range(4):
            tr(PSB(7)[:, qs * 128:(qs + 1) * 128], asb[:, qs, :], identB, reads=[("asb", ia, qs), "cstB"], writes=[("ps", 7)])
        A("dve", lambda e: e.tensor_copy(out=stg, in_=PSB(7)[:, 0:512]), reads=[("ps", 7)], writes=[("stg", ia)])
        dma("sp", dst_d[h, :, g * 512:(g + 1) * 512], stg, [("stg", ia)], [("atd", id(dst_d), h, g)], ("atst", ia))

    for g in range(2):
        NKT = 16 if g == 0 else 32
        NK = NKT * 128
        AR.reset()
        QiG = AR.alloc([8, 512], BF16)
        kposB = AR.alloc([NK], F32)
        scores = [AR.alloc([NK], F32) for _ in range(2)]
        junks = [AR.alloc([NK], BF16) for _ in range(2)]
        m01s = [AR.alloc([NK], BF16) for _ in range(2)]
        maskT = AR.alloc([NKT, 512], BF16)
        bsts = [AR.alloc([64], F32) for _ in range(2)]
        QbG = AR.alloc([8, 512], BF16)
        Kh = [AR.alloc([NK], BF16) for _ in range(2)]
        Vh = [AR.alloc([NKT, 129], BF16) for _ in range(2)]
        esb = [AR.alloc([512], BF16) for _ in range(2)]
        pT = [AR.alloc([512], BF16) for _ in range(2)]
        asb = [AR.alloc([4, 128], BF16) for _ in range(2)]
        stg = [AR.alloc([512], BF16) for _ in range(2)]
        dma("sp", QiG, QiT_d[:, :, g * 512:(g + 1) * 512].rearrange("h p t -> p h t"), [], ["QiG"], "QiG")
        dma("sp", kposB, kpos_d[:, 0:NK], [], ["kposB"], "kposB")
        dma("sp", QbG, QbT_d[:, :, g * 512:(g + 1) * 512].rearrange("h p t -> p h t"), [], ["QbG"], "QbG")
        cnt = [0]
        NB = N_BISECT
        for qp in range(2):
            for z in range(2):
                qt = qp * 2 + z
                Tq = 4 * g + qt
                score = scores[z]
                for c in range(NKT // 4):
                    scr = ("score", z, c)
                    sc = score[:, c * 512:(c + 1) * 512]
                    for h in range(16):
                        half, pair = h % 2, h // 2
                        bs_, br_ = cnt[0] % 2, 2 + (cnt[0] % 2)
                        cnt[0] += 1
                        mm(PS(bs_), QiG[64 * half:64 * half + 64, pair, qt * 128:(qt + 1) * 128],
                           kiT2[64 * half:64 * half + 64, c * 512:(c + 1) * 512], True, True,
                           reads=["QiG", "kiT2"], writes=[("ps", bs_)])
                        col = Tq * 16 + h
                        A("act", lambda e, bs_=bs_, br_=br_, col=col: e.activation(
                            out=PS(br_), in_=PS(bs_), func=AF.Relu, scale=wabs[:, col:col + 1]),
                          reads=[("ps", bs_), "wabs"], writes=[("ps", br_)])
                        if h == 0:
                            A("dve", lambda e, br_=br_, col=col, sc=sc: e.tensor_scalar(
                                out=sc, in0=PS(br_), scalar1=wsgn[:, col:col + 1], scalar2=None, op0=ALU.mult),
                              reads=[("ps", br_), "wsgn"], writes=[scr])
                        else:
                            A("dve", lambda e, br_=br_, col=col, sc=sc: e.scalar_tensor_tensor(
                                out=sc, in0=PS(br_), scalar=wsgn[:, col:col + 1], in1=sc, op0=ALU.mult, op1=ALU.add),
                              reads=[("ps", br_), "wsgn", scr], writes=[scr])
            for z in range(2):
                qt = qp * 2 + z
                Tq = 4 * g + qt
                score, junk, bst = scores[z], junks[z], bsts[z]
                allsc = [("score", z, c) for c in range(NKT // 4)]
                rmx, rmn, w0, mid, cntc, uu, lo = [bst[:, i:i + 1] for i in range(7)]
                tab = bst[:, 8:8 + NB + 1]
                B_ = lambda i, z=z: ("bst", z, i)
                A("dve", lambda e: e.tensor_reduce(out=rmx, in_=score, axis=AX.X, op=ALU.max), reads=allsc, writes=[B_(0)])
                A("dve", lambda e: e.tensor_reduce(out=rmn, in_=score, axis=AX.X, op=ALU.min), reads=allsc, writes=[B_(1)])
                A("dve", lambda e: e.tensor_tensor(out=w0, in0=rmx, in1=rmn, op=ALU.subtract), reads=[B_(0), B_(1)], writes=[B_(2)])
                A("dve", lambda e: e.tensor_scalar(out=w0, in0=w0, scalar1=1.001, scalar2=1e-20, op0=ALU.mult, op1=ALU.add),
                  reads=[B_(2)], writes=[B_(2)])
                A("dve", lambda e: e.tensor_scalar(out=tab, in0=ftab[:, 0:NB + 1], scalar1=w0, scalar2=None, op0=ALU.mult),
                  reads=[B_(2), "cstF"], writes=[B_(8)])
                A("dve", lambda e: e.tensor_tensor(out=mid, in0=rmn, in1=tab[:, 0:1], op=ALU.add), reads=[B_(1), B_(8)], writes=[B_(3)])
                A("dve", lambda e, Tq=Tq: e.tensor_scalar(out=junk, in0=kposB, scalar1=qposT[:, Tq:Tq + 1], scalar2=None,
                                                          op0=ALU.is_gt), reads=["kposB", "cstF"], writes=[("junk", z)])
                A("dve", lambda e: e.scalar_tensor_tensor(out=score, in0=junk, scalar=-1.0e9, in1=score, op0=ALU.mult, op1=ALU.add),
                  reads=allsc + [("junk", z)], writes=allsc)
            for it in range(1, NB + 1):
                for z in range(2):
                    score, junk, bst = scores[z], junks[z], bsts[z]
                    allsc = [("score", z, c) for c in range(NKT // 4)]
                    rmx, rmn, w0, mid, cntc, uu, lo = [bst[:, i:i + 1] for i in range(7)]
                    tab = bst[:, 8:8 + NB + 1]
                    B_ = lambda i, z=z: ("bst", z, i)
                    A("dve", lambda e: e.tensor_scalar(out=junk, in0=score, scalar1=mid, scalar2=None, op0=ALU.is_ge, op1=ALU.add,
                                                       accum_out=cntc), reads=allsc + [B_(3)], writes=[("junk", z), B_(4)])
                    A("dve", lambda e, it=it: e.tensor_scalar(out=uu, in0=cntc, scalar1=255.5, scalar2=tab[:, it - 1:it],
                                                             op0=ALU.is_ge, op1=ALU.mult), reads=[B_(4), B_(8)], writes=[B_(5)])
                    A("dve", lambda e, it=it: e.tensor_scalar(out=mid, in0=mid, scalar1=tab[:, it:it + 1], scalar2=uu,
                                                             op0=ALU.subtract, op1=ALU.add), reads=[B_(3), B_(5), B_(8)], writes=[B_(3)])
            for z in range(2):
                qt = qp * 2 + z
                score, junk, bst, m01 = scores[z], junks[z], bsts[z], m01s[z]
                allsc = [("score", z, c) for c in range(NKT // 4)]
                rmx, rmn, w0, mid, cntc, uu, lo = [bst[:, i:i + 1] for i in range(7)]
                tab = bst[:, 8:8 + NB + 1]
                B_ = lambda i, z=z: ("bst", z, i)
                A("dve", lambda e: e.tensor_tensor(out=lo, in0=mid, in1=tab[:, NB:NB + 1], op=ALU.subtract),
                  reads=[B_(3), B_(8)], writes=[B_(6)])
                A("dve", lambda e: e.tensor_scalar(out=m01, in0=score, scalar1=lo, scalar2=None, op0=ALU.is_ge),
                  reads=allsc + [B_(6)], writes=[("m01", z)])
                for k8 in range(NKT // 8):
                    bank = 4 + (k8 % 2)
                    for j in range(8):
                        kt = k8 * 8 + j
                        tr(PSB(bank)[:, j * 128:(j + 1) * 128], m01[:, kt * 128:(kt + 1) * 128], identB,
                           reads=[("m01", z), "cstB"], writes=[("ps", bank)])
                    A("act", lambda e, bank=bank, k8=k8, qt=qt: e.activation(
                        out=maskT[:, k8 * 8:(k8 + 1) * 8, qt * 128:(qt + 1) * 128],
                        in_=PSB(bank).rearrange("p (j q) -> p j q", j=8), func=AF.Copy),
                      reads=[("ps", bank)], writes=[("maskT", qt)])
        maskT_res = [("maskT", qt) for qt in range(4)]
        if stop == 6:
            dma("sp", dbg_d[:, 0:NKT * 256].bitcast(BF16) if False else x1_d[0:128, :].bitcast(BF16)[:, 0:4096].rearrange("p (a b) -> p a b", a=8)[:, :, :],
                maskT[:, 0:8, :], maskT_res, ["dbgm"], "dbgm") if False else None
            P.barrier()
            return nc, st, P

        if stop == 61:
            P.barrier()
            continue
        def loadKV(h, kd, vd, pref):
            i = h % 2
            dma("sp", Kh[i], kd[h, :, 0:NK], [], [(pref + "K", i)], (pref + "K", i))
            dma("sp", Vh[i], vd[h, :, 0:NKT, :], [], [(pref + "V", i)], (pref + "V", i))

        loadKV(0, kTb_d, vb_d, "b")
        for h in range(8):
            if h + 1 < 8:
                loadKV(h + 1, kTb_d, vb_d, "b")
            i = h % 2
            for kt in range(NKT):
                bs_ = kt % 2
                mm(PS(bs_), Kh[i][:, kt * 128:(kt + 1) * 128], QbG[:, h, :], True, True,
                   reads=[("bK", i), "QbG"], writes=[("ps", bs_)])
                A("act", lambda e, bs_=bs_, kt=kt: e.activation(out=esb[kt % 2], in_=PS(bs_), func=AF.Exp, scale=SCALE),
                  reads=[("ps", bs_)], writes=[("esb", kt % 2)])
                A("dve", lambda e, kt=kt: e.tensor_tensor(out=pT[kt % 2], in0=esb[kt % 2], in1=maskT[:, kt, :], op=ALU.mult),
                  reads=[("esb", kt % 2)] + maskT_res, writes=[("pT", kt % 2)])
                for qs in range(4):
                    mm(psb[2 + qs][:, 0:129], pT[kt % 2][:, qs * 128:(qs + 1) * 128], Vh[i][:, kt, :], kt == 0, kt == NKT - 1,
                       reads=[("pT", kt % 2), ("bV", i)], writes=[("ps", 2 + qs)])
            finalize_heads(h, g, asb[h % 2], stg[h % 2], atTb_d, h % 2)
        P.barrier()
    if stop == 7:
        return nc, st, P

    AR.reset()
    QaT = AR.alloc([8, 1024], BF16)
    KaTo = AR.alloc([8, 1024], BF16)
    Vao = AR.alloc([8, 8, 129], BF16)
    MT = AR.alloc([8, 1024], BF16)
    gsc = AR.alloc([6, 128], F32)
    m8 = AR.alloc([64], F32)
    Mq = AR.alloc([128], BF16)
    Kh = [AR.alloc([4096], BF16) for _ in range(2)]
    Vh = [AR.alloc([32, 129], BF16) for _ in range(2)]
    pT = [AR.alloc([512], BF16) for _ in range(2)]
    eo = [AR.alloc([128], BF16) for _ in range(2)]
    asb = [AR.alloc([4, 128], BF16) for _ in range(2)]
    stg = [AR.alloc([512], BF16) for _ in range(2)]
    dma("sp", QaT, QaT_d.rearrange("h p t -> p h t"), [], ["QaT"], "QaT")
    dma("sp", KaTo, KaTo_d.rearrange("h p t -> p h t"), [], ["KaTo"], "KaTo")
    dma("sp", Vao, Vao_d, [], ["Vao"], "Vao")
    pastm, negm, gm, sel = gsc[:, 0, :], gsc[:, 1, :], gsc[:, 2, :], gsc[:, 3, :]
    A("dve", lambda e: e.memset(MT, 0.0), writes=[("MT", q) for q in range(8)])
    for Tq in range(8):
        for h in range(8):
            mm(psb[7][:, h * 16:(h + 1) * 16], QaT[:, h, Tq * 128:(Tq + 1) * 128], kmeanT[:, h * 16:(h + 1) * 16], True, True,
               reads=["QaT", "kmeanT"], writes=[("ps", 7)])
        A("dve", lambda e, Tq=Tq: e.tensor_scalar(out=pastm, in0=blockend, scalar1=qposT[:, Tq:Tq + 1], scalar2=None, op0=ALU.is_le),
          reads=["cstF"], writes=["pastm"])
        A("dve", lambda e: e.tensor_scalar(out=negm, in0=pastm, scalar1=1.0, scalar2=BIGM, op0=ALU.subtract, op1=ALU.mult),
          reads=["pastm"], writes=["negm"])
        A("dve", lambda e: e.tensor_tensor(out=gm, in0=psb[7][:, 0:128], in1=negm, op=ALU.add),
          reads=[("ps", 7), "negm"], writes=["gm"])
        for h in range(8):
            A("dve", lambda e, h=h: e.max(out=m8[:, h * 8:(h + 1) * 8], in_=gm[:, h * 16:(h + 1) * 16]),
              reads=["gm"], writes=[("m8", h)])
        for h in range(8):
            A("dve", lambda e, h=h: e.tensor_scalar(out=sel[:, h * 16:(h + 1) * 16], in0=gm[:, h * 16:(h + 1) * 16],
                                                     scalar1=m8[:, h * 8 + 2:h * 8 + 3], scalar2=None, op0=ALU.is_ge),
              reads=["gm", ("m8", h)], writes=[("sel", h)])
        A("dve", lambda e: e.tensor_tensor(out=sel, in0=sel, in1=pastm, op=ALU.mult),
          reads=[("sel", h) for h in range(8)] + ["pastm"], writes=[("sel", h) for h in range(8)])
        A("dve", lambda e: e.tensor_scalar(out=Mq, in0=sel, scalar1=1.0, scalar2=BIGM, op0=ALU.subtract, op1=ALU.mult),
          reads=[("sel", h) for h in range(8)], writes=["Mq"])
        for h in range(8):
            tr(PSB(6)[0:16, h * 128:(h + 1) * 128], Mq[:, h * 16:(h + 1) * 16], identB, reads=["Mq", "cstB"], writes=[("ps", 6)])
        A("act", lambda e, Tq=Tq: e.activation(out=MT[0:16, :, Tq * 128:(Tq + 1) * 128],
                                               in_=PSB(6)[0:16, :].rearrange("p (h q) -> p h q", h=8), func=AF.Copy),
          reads=[("ps", 6)], writes=[("MT", Tq)])
    for g in range(2):
        NKT = 16 if g == 0 else 32
        NK = NKT * 128
        MT_res = [("MT", 4 * g + q) for q in range(4)]

        def loadKVa(h):
            i = h % 2
            dma("sp", Kh[i][:, 0:NK], kTa_d[h, :, 0:NK], [], [("aK", i)], ("aK", i))
            dma("sp", Vh[i][:, 0:NKT, :], va_d[h, :, 0:NKT, :], [], [("aV", i)], ("aV", i))

        loadKVa(0)
        ecnt = [0]
        for h in range(8):
            if h + 1 < 8:
                loadKVa(h + 1)
            i = h % 2
            for kt in range(NKT):
                bs_ = kt % 2
                mm(PS(bs_), Kh[i][:, kt * 128:(kt + 1) * 128], QaT[:, h, g * 512:(g + 1) * 512], True, False,
                   reads=[("aK", i), "QaT"], writes=[("ps", bs_)])
                n0 = kt // 2
                mm(PS(bs_), eselB[:, n0 * 128:(n0 + 1) * 128], MT[:, h, g * 512:(g + 1) * 512], False, True,
                   reads=["eselB"] + MT_res, writes=[("ps", bs_)])
                A("act", lambda e, bs_=bs_, kt=kt: e.activation(out=pT[kt % 2], in_=PS(bs_), func=AF.Exp, scale=SCALE),
                  reads=[("ps", bs_)], writes=[("pT", kt % 2)])
                for qs in range(4):
                    mm(psb[2 + qs][:, 0:129], pT[kt % 2][:, qs * 128:(qs + 1) * 128], Vh[i][:, kt, :], kt == 0, False,
                       reads=[("pT", kt % 2), ("aV", i)], writes=[("ps", 2 + qs)])
            for qs in range(4):
                Tq = 4 * g + qs
                tiles = [(Tq, True)] if Tq % 2 == 0 else [(Tq - 1, False), (Tq, True)]
                for ti, (ot, diag) in enumerate(tiles):
                    ei = ecnt[0] % 2
                    ecnt[0] += 1
                    mm(psb[6][:, 0:128], KaTo[:, h, ot * 128:(ot + 1) * 128], QaT[:, h, Tq * 128:(Tq + 1) * 128], True, True,
                       reads=["KaTo", "QaT"], writes=[("ps", 6)])
                    A("act", lambda e, ei=ei: e.activation(out=eo[ei], in_=psb[6][:, 0:128], func=AF.Exp, scale=SCALE),
                      reads=[("ps", 6)], writes=[("eo", ei)])
                    if diag:
                        A("dve", lambda e, ei=ei: e.tensor_tensor(out=eo[ei], in0=eo[ei], in1=triB, op=ALU.mult),
                          reads=[("eo", ei), "cstB"], writes=[("eo", ei)])
                    mm(psb[2 + qs][:, 0:129], eo[ei], Vao[:, ot, h, :], False, ti == len(tiles) - 1,
                       reads=[("eo", ei), "Vao"], writes=[("ps", 2 + qs)])
            finalize_heads(h, g, asb[h % 2], stg[h % 2], atTa_d, h % 2)
    P.barrier()
    if stop == 8:
        return nc, st, P

    def rs_from_ss(ss, rss):
        vv, rvv = smcol()
        rs, rrs = smcol()
        A("act", lambda e: e.activation(out=vv, in_=ss, func=AF.Ln, scale=1.0 / D, bias=epsc), reads=[rss, "neghalf"], writes=[rvv])
        A("act", lambda e: e.activation(out=rs, in_=vv, func=AF.Exp, scale=-0.5), reads=[rvv], writes=[rrs])
        return rs, rrs

    AR.reset()
    atA = AR.alloc([8, 512], BF16)
    atB = AR.alloc([8, 512], BF16)
    hTo = AR.alloc([16, 512], BF16)
    ycT = AR.alloc([16, 512], BF16)
    wso = WStream("wso", 2, [8, 512])
    wsg = WStream("wsg", 2, [16, 512])
    sg = [AR.alloc([512], F32) for _ in range(4)]
    ysb = AR.alloc([4, 2048], F32)
    GTB = AR.alloc([2048], F32)
    xt6 = [AR.alloc([2048], F32) for _ in range(2)]
    junk6 = AR.alloc([2048], BF16)
    dma("sp", GTB, gt_d[0].partition_broadcast(128), [], ["GTB"], "GTB")
    for g in range(2):
        gs = slice(g * 512, (g + 1) * 512)
        dma("sp", atA, atTa_d[:, :, gs].rearrange("h p t -> p h t"), [], ["atA"], "atA")
        dma("sp", atB, atTb_d[:, :, gs].rearrange("h p t -> p h t"), [], ["atB"], "atB")
        dma("sp", hTo, hTo_d[:, :, gs], [], ["hTo"], "hTo")
        for cc in range(4):
            wmo, rmo = wso.load(w_mo[:, cc * 512:(cc + 1) * 512].rearrange("(h p) n -> p h n", p=128))
            wdo, rdo = wso.load(w_do[:, cc * 512:(cc + 1) * 512].rearrange("(h p) n -> p h n", p=128))
            wga, rga = wsg.load(wsrc(w_in, C_GA + cc * 512, 512))
            wgb, rgb = wsg.load(wsrc(w_in, C_GB + cc * 512, 512))
            for ci in range(4):
                ct = cc * 4 + ci
                b0 = 4 * (ct % 2)
                cs_ = slice(ci * 128, (ci + 1) * 128)
                for h in range(8):
                    mm(PS(b0), wmo[:, h, cs_], atA[:, h, :], h == 0, h == 7, reads=[rmo, "atA"], writes=[("ps", b0)])
                for h in range(8):
                    mm(PS(b0 + 1), wdo[:, h, cs_], atB[:, h, :], h == 0, h == 7, reads=[rdo, "atB"], writes=[("ps", b0 + 1)])
                for kc in range(16):
                    mm(PS(b0 + 2), wga[:, kc, cs_], hTo[:, kc, :], kc == 0, kc == 15, reads=[rga, "hTo"], writes=[("ps", b0 + 2)])
                for kc in range(16):
                    mm(PS(b0 + 3), wgb[:, kc, cs_], hTo[:, kc, :], kc == 0, kc == 15, reads=[rgb, "hTo"], writes=[("ps", b0 + 3)])
                s0, s1 = sg[2 * (ct % 2)], sg[2 * (ct % 2) + 1]
                r0, r1 = ("sg", 2 * (ct % 2)), ("sg", 2 * (ct % 2) + 1)
                A("act", lambda e, b0=b0, s0=s0: e.activation(out=s0, in_=PS(b0 + 2), func=AF.Sigmoid), reads=[("ps", b0 + 2)], writes=[r0])
                A("act", lambda e, b0=b0, s1=s1: e.activation(out=s1, in_=PS(b0 + 3), func=AF.Sigmoid), reads=[("ps", b0 + 3)], writes=[r1])
                A("dve", lambda e, b0=b0, s0=s0: e.tensor_tensor(out=s0, in0=PS(b0), in1=s0, op=ALU.mult), reads=[("ps", b0), r0], writes=[r0])
                A("dve", lambda e, b0=b0, s1=s1: e.tensor_tensor(out=s1, in0=PS(b0 + 1), in1=s1, op=ALU.mult), reads=[("ps", b0 + 1), r1], writes=[r1])
                A("dve", lambda e, ct=ct, s0=s0, s1=s1: e.tensor_tensor(out=ycT[:, ct, :], in0=s0, in1=s1, op=ALU.add),
                  reads=[r0, r1], writes=[("ycT", ct)])
        yc_res = [("ycT", ct) for ct in range(16)]
        ycnt = [0]
        for cc in range(4):
            wo_, rwo = wsg.load(wsrc(w_o, cc * 512, 512))
            for tt in range(4):
                bank = ycnt[0] % 2
                ycnt[0] += 1
                for kc in range(16):
                    mm(PS(bank), ycT[:, kc, tt * 128:(tt + 1) * 128], wo_[:, kc, :], kc == 0, kc == 15,
                       reads=[rwo] + yc_res, writes=[("ps", bank)])
                A("act", lambda e, bank=bank, tt=tt, cc=cc: e.activation(out=ysb[:, tt, cc * 512:(cc + 1) * 512], in_=PS(bank), func=AF.Copy),
                  reads=[("ps", bank)], writes=[("ysb", tt)])
        for tt in range(4):
            row0 = g * 512 + tt * 128
            xt = xt6[tt % 2]
            rx = ("xt6", tt % 2)
            dma("sp", xt, xq[row0:row0 + 128, :], [], [rx], rx)
            ss, rss = smcol()
            A("act", lambda e, tt=tt, ss=ss: e.activation(out=junk6, in_=ysb[:, tt, :], func=AF.Square, accum_out=ss),
              reads=[("ysb", tt)], writes=["junk6", rss])
            rs, rrs = rs_from_ss(ss, rss)
            A("dve", lambda e, tt=tt, rs=rs: e.scalar_tensor_tensor(out=ysb[:, tt, :], in0=ysb[:, tt, :], scalar=rs, in1=GTB,
                                                                     op0=ALU.mult, op1=ALU.mult),
              reads=[("ysb", tt), rrs, "GTB"], writes=[("ysb", tt)])
            A("dve", lambda e, tt=tt, xt=xt: e.tensor_tensor(out=xt, in0=ysb[:, tt, :], in1=xt, op=ALU.add),
              reads=[("ysb", tt), rx], writes=[rx])
            dma("sp", x1_d[row0:row0 + 128, :], xt, [rx], [("x1d", row0)], ("x1st", tt % 2))
    P.barrier()
    if stop == 9:
        return nc, st, P

    for g in range(2):
        AR.reset()
        h2T = AR.alloc([16, 512], BF16)
        uT = AR.alloc([64, 512], BF16)
        mark = AR.off
        nctx = norm_setup()
        for tt in range(4):
            row0 = g * 512 + tt * 128
            norm_tile(nctx, x1_d[row0:row0 + 128, :], G2T, S2T,
                      (lambda kc, tt=tt: h2T[:, kc, tt * 128:(tt + 1) * 128]), (("h2T", tt, 0), ("h2T", tt, 1)))
        P.barrier()
        AR.off = mark
        wsf = WStream("wsf", 2, [16, 512])
        rsb = [AR.alloc([512], F32) for _ in range(2)]
        fsb = AR.alloc([4, 2048], F32)
        GT2B = AR.alloc([2048], F32)
        xt7 = [AR.alloc([2048], F32) for _ in range(2)]
        junk7 = AR.alloc([2048], BF16)
        dma("sp", GT2B, gt_d[1].partition_broadcast(128), [], ["GT2B"], "GT2B")
        for fc in range(16):
            w1, rw1 = wsf.load(wsrc(w_ff1, fc * 512, 512))
            for fi in range(4):
                ft = fc * 4 + fi
                bank = 2 + (ft % 2)
                for kc in range(16):
                    mm(PS(bank), w1[:, kc, fi * 128:(fi + 1) * 128], h2T[:, kc, :], kc == 0, kc == 15,
                       reads=[rw1, "h2Tall"], writes=[("ps", bank)])
                rr = rsb[ft % 2]
                A("act", lambda e, bank=bank, rr=rr: e.activation(out=rr, in_=PS(bank), func=AF.Relu),
                  reads=[("ps", bank)], writes=[("rsb", ft % 2)])
                A("dve", lambda e, bank=bank, rr=rr, ft=ft: e.scalar_tensor_tensor(
                    out=uT[:, ft, :], in0=PS(bank), scalar=0.0, in1=rr, op0=ALU.max, op1=ALU.mult),
                  reads=[("ps", bank), ("rsb", ft % 2)], writes=[("uT", ft)])
        for cc in range(4):
            for fq in range(4):
                w2, rw2 = wsf.load(w_ff2[fq * 2048:(fq + 1) * 2048, cc * 512:(cc + 1) * 512].rearrange("(ft p) n -> p ft n", p=128))
                for fi in range(16):
                    ft = fq * 16 + fi
                    for tt in range(4):
                        mm(PS(4 + tt), uT[:, ft, tt * 128:(tt + 1) * 128], w2[:, fi, :], ft == 0, ft == 63,
                           reads=[rw2, ("uT", ft)], writes=[("ps", 4 + tt)])
            for tt in range(4):
                A("act", lambda e, tt=tt, cc=cc: e.activation(out=fsb[:, tt, cc * 512:(cc + 1) * 512], in_=PS(4 + tt), func=AF.Copy),
                  reads=[("ps", 4 + tt)], writes=[("fsb", tt)])
        for tt in range(4):
            row0 = g * 512 + tt * 128
            xt = xt7[tt % 2]
            rx = ("xt7", tt % 2)
            dma("sp", xt, x1_d[row0:row0 + 128, :], [], [rx], rx)
            ss, rss = smcol()
            A("act", lambda e, tt=tt, ss=ss: e.activation(out=junk7, in_=fsb[:, tt, :], func=AF.Square, accum_out=ss),
              reads=[("fsb", tt)], writes=["junk7", rss])
            rs, rrs = rs_from_ss(ss, rss)
            A("dve", lambda e, tt=tt, rs=rs: e.scalar_tensor_tensor(out=fsb[:, tt, :], in0=fsb[:, tt, :], scalar=rs, in1=GT2B,
                                                                     op0=ALU.mult, op1=ALU.mult),
              reads=[("fsb", tt), rrs, "GT2B"], writes=[("fsb", tt)])
            A("dve", lambda e, tt=tt, xt=xt: e.tensor_tensor(out=xt, in0=fsb[:, tt, :], in1=xt, op=ALU.add),
              reads=[("fsb", tt), rx], writes=[rx])
            dma("sp", out_d[row0:row0 + 128, :], xt, [rx], [("outd", row0)], ("ost", tt % 2))
        P.barrier()
    return nc, st, P


def _rope_tables(pos):
    pos = np.asarray(pos, np.float32)
    out = np.zeros((4, 128, pos.shape[0]), np.float32)
    for which, half in ((0, 64), (2, 32)):
        inv = np.power(np.float32(10000.0), -np.arange(half, dtype=np.float32) / np.float32(half)).astype(np.float32)
        ang = (pos[None, :] * inv[:, None]).astype(np.float32)
        c = np.cos(ang).astype(np.float32)
        s = np.sin(ang).astype(np.float32)
        d = np.arange(128)
        dd = d % (2 * half)
        idx = dd % half
        sign = np.where(dd < half, -1.0, 1.0).astype(np.float32)
        out[which] = c[idx]
        out[which + 1] = s[idx] * sign[:, None]
    return out


def _consts(qpos):
    c = np.zeros((128, 5 * 128 + 8 + 32), np.float32)
    c[:, 0:128] = np.eye(128, dtype=np.float32)
    m = np.arange(128)
    p128 = np.zeros((128, 128), np.float32)
    p128[(m + 64) % 128, m] = 1.0
    c[:, 128:256] = p128
    p64 = np.zeros((128, 128), np.float32)
    p64[(m // 64) * 64 + ((m % 64) + 32) % 64, m] = 1.0
    c[:, 256:384] = p64
    k = np.arange(128)[:, None]
    q = np.arange(128)[None, :]
    c[:, 384:512] = (k <= q).astype(np.float32)
    n = np.arange(128) % 16
    c[:, 512:640] = ((n + 1) * 256).astype(np.float32)[None, :]
    c[:, 640:648] = qpos.reshape(8, 128).T
    c[:, 648:680] = (2.0 ** -(np.arange(32, dtype=np.float64) + 1)).astype(np.float32)[None, :]
    return c


def prep_inputs(core, x, c, w_ada, b_ada, g_pre_mix, g_post_mix, w_in, w_moba_out, w_dsa_out, w_o,
                g_pre_ffn, g_post_ffn, w_ff1, w_ff2, shared=None):
    b, j = core // 4, core % 4
    blkA, blkB = j, 7 - j
    own = np.concatenate([np.arange(blkA * 512, blkA * 512 + 512), np.arange(blkB * 512, blkB * 512 + 512)])
    qpos = own.astype(np.float32)
    f = lambda a: np.ascontiguousarray(a, dtype=np.float32)
    tcol = lambda v, n: f(np.asarray(v).reshape(n, 128).T)
    if shared is None:
        shared = {}
    if "w_ada" not in shared:
        esel = np.zeros((128, 16, 128), np.float32)
        for n0 in range(16):
            esel[n0, n0, :] = 1.0
        shared.update(
            w_ada=f(w_ada[0]), b_adaT=tcol(b_ada[0], 96),
            gT=f(np.concatenate([tcol(g_pre_mix[0], 16), tcol(g_post_mix[0], 16),
                                 tcol(g_pre_ffn[0], 16), tcol(g_post_ffn[0], 16)], axis=1)),
            w_in=f(w_in[0]), w_ki2=f(np.concatenate([w_in[0][:, C_KI:C_KI + 64], w_in[0][:, C_KI:C_KI + 64]], axis=1)),
            w_mo=f(w_moba_out[0]), w_do=f(w_dsa_out[0]), w_o=f(w_o[0]), w_ff1=f(w_ff1[0]), w_ff2=f(w_ff2[0]),
            ropeF=_rope_tables(np.arange(T, dtype=np.float32)),
            esel=f(esel.reshape(128, 16 * 128)),
            kposB=f(np.broadcast_to(np.arange(T, dtype=np.float32)[None, :], (128, T))),
        )
    m = dict(shared)
    m.update(
        xfull=f(x[b]), xq=f(x[b][own]), cT=tcol(c[b], 16),
        ropeQ=_rope_tables(qpos), cst=_consts(qpos),
    )
    return m, b, own


_CACHE = {}


def kernel(**inputs):
    inputs = {k: np.asarray(v) for k, v in inputs.items()}
    if "nc" not in _CACHE:
        nc, st, P = build()
        P.emit(st)
        st.close()
        _CACHE["nc"] = nc
    nc = _CACHE["nc"]
    shared = {}
    in_maps, owns = [], []
    for core in range(8):
        m, b, own = prep_inputs(core, shared=shared, **inputs)
        in_maps.append(m)
        owns.append((b, own))
    res = run_bass_kernel_spmd(nc, in_maps, core_ids=list(range(8)))
    out = np.empty((2, T, D), np.float32)
    for core in range(8):
        b, own = owns[core]
        out[b, own] = np.asarray(res.results[core]["out"])
    return out
```

```python
import numpy as np
from contextlib import ExitStack
import concourse.bass as bass
import concourse.mybir as mybir
from concourse.bass_utils import run_bass_kernel_spmd

F32 = mybir.dt.float32
BF16 = mybir.dt.bfloat16
U32 = mybir.dt.uint32
ALU = mybir.AluOpType
AF = mybir.ActivationFunctionType
AX = mybir.AxisListType

ENGS = ("pe", "act", "dve", "pool", "sp")


class Op:
    __slots__ = ("eng", "fn", "deps", "sig", "cnt", "sem", "is_dma", "idx", "nosync_same")

    def __init__(self, eng, fn, is_dma):
        self.eng = eng
        self.fn = fn
        self.is_dma = is_dma
        self.deps = []
        self.sig = False
        self.cnt = 0
        self.sem = None
        self.idx = -1
        self.nosync_same = False


class _Rec:
    def __getattr__(self, name):
        def f(*a, **k):
            self.call = (name, a, k)
            return self
        return f


class Prog:
    def __init__(self, nc):
        self.nc = nc
        self.ops = {e: [] for e in ENGS}
        self.last_w = {}
        self.readers = {}
        self.dma_keys = {}
        self.n_ops = 0

    def _collect(self, op, reads, writes):
        deps = {}

        def add_dep(d):
            if d is None or d is op:
                return
            if d.is_dma:
                deps[("dma", id(d))] = d
            else:
                k = ("eng", d.eng)
                if k not in deps or deps[k].idx < d.idx:
                    deps[k] = d

        for r in reads:
            add_dep(self.last_w.get(r))
        for r in writes:
            add_dep(self.last_w.get(r))
            rd = self.readers.get(r)
            if rd:
                for d in rd.values():
                    add_dep(d)
        return deps

    def add(self, eng, fn, reads=(), writes=(), dma_key=None, pe_acc=False):
        is_dma = dma_key is not None
        rec = _Rec()
        fn(rec)
        _name, _a, _k = rec.call
        fn = (lambda eng, _name=_name, _a=_a, _k=_k: getattr(eng, _name)(*_a, **_k))
        writes = list(writes) + [r for r in reads if isinstance(r, tuple) and r[0] == "ps" and r not in writes]
        op = Op(eng, fn, is_dma)
        op.idx = len(self.ops[eng])
        op.nosync_same = pe_acc
        deps = self._collect(op, reads, writes)
        if is_dma:
            ent = self.dma_keys.setdefault(dma_key, [None, None, 0])
            if ent[1] is not None:
                deps[("dma", id(ent[1]))] = ent[1]
            ent[2] += 16
            op.cnt = ent[2]
            op.sem = dma_key
            ent[1] = op
        dl = []
        for d in deps.values():
            if (not d.is_dma) and d.eng == eng and (pe_acc or eng in ("sp", "pe")):
                continue
            dl.append(d)
            d.sig = True
        op.deps = dl
        self.ops[eng].append(op)
        for r in reads:
            rd = self.readers.setdefault(r, {})
            rd[("dma", id(op)) if is_dma else ("eng", eng)] = op
        for r in writes:
            self.last_w[r] = op
            self.readers[r] = {}
        self.n_ops += 1
        return op

    def emit(self, stack, final_waits=()):
        nc = self.nc
        esem = {e: stack.enter_context(nc.semaphore("s_" + e)) for e in ENGS}
        for i, (k, ent) in enumerate(self.dma_keys.items()):
            ent[0] = stack.enter_context(nc.semaphore("d%d" % i))
        for e in ENGS:
            c = 0
            for op in self.ops[e]:
                if op.is_dma:
                    continue
                if op.sig:
                    c += 1
                    op.cnt = c
        block = stack.enter_context(nc.Block())

        def run(e, eng):
            known = {}
            for op in self.ops[e]:
                for d in op.deps:
                    if d.is_dma:
                        s = self.dma_keys[d.sem][0]
                        key = ("d", d.sem)
                    else:
                        s = esem[d.eng]
                        key = ("e", d.eng)
                    if known.get(key, 0) >= d.cnt:
                        continue
                    eng.wait_ge(s, d.cnt)
                    known[key] = d.cnt
                if op.fn is None:
                    continue
                ins = op.fn(eng)
                if op.is_dma:
                    ins.then_inc(self.dma_keys[op.sem][0], 16)
                elif op.sig:
                    ins.then_inc(esem[e], 1)
            if e == "sp":
                for k in final_waits:
                    ent = self.dma_keys[k]
                    eng.wait_ge(ent[0], ent[2])

        @block.tensor
        def _(eng):
            run("pe", eng)

        @block.scalar
        def _(eng):
            run("act", eng)

        @block.vector
        def _(eng):
            run("dve", eng)

        @block.gpsimd
        def _(eng):
            run("pool", eng)

        @block.sync
        def _(eng):
            run("sp", eng)

    def barrier(self):
        lasts = []
        for e in ENGS:
            for op in reversed(self.ops[e]):
                if not op.is_dma and op.fn is not None:
                    lasts.append(op)
                    break
        for ent in self.dma_keys.values():
            if ent[1] is not None:
                lasts.append(ent[1])
        for e in ENGS:
            op = Op(e, None, False)
            op.idx = len(self.ops[e])
            dl = []
            for d in lasts:
                if (not d.is_dma) and d.eng == e and e in ("pe", "sp"):
                    continue
                d.sig = True
                dl.append(d)
            op.deps = dl
            self.ops[e].append(op)
        self.last_w = {}
        self.readers = {}


D = 2048
T = 4096
NOWN = 1024
DIN = 11344
DFF = 8192
BIGM = 30000.0
SCALE = 128.0 ** -0.5
C_QA, C_KA, C_VA, C_QB, C_KB, C_VB, C_QI, C_KI, C_WI, C_GA, C_GB = (
    0, 1024, 2048, 3072, 4096, 5120, 6144, 7168, 7232, 7248, 9296)
ARENA_BYTES = 176 * 1024
N_BISECT = 18


class Arena:
    def __init__(self, t, nbytes):
        self.t = t
        self.n = nbytes
        self.off = 0

    def reset(self):
        self.off = 0

    def alloc(self, shape, dt):
        n = 1
        for s in shape:
            n *= s
        esz = 4 if dt == F32 else 2
        nb = (n * esz + 63) // 64 * 64
        o = self.off
        self.off += nb
        assert self.off <= self.n, ("arena overflow", self.off, self.n)
        ap = self.t[:, o // 2:(o + n * esz) // 2]
        if dt == F32:
            ap = ap.bitcast(F32)
        if len(shape) == 2:
            ap = ap.rearrange("p (a b) -> p a b", a=shape[0])
        elif len(shape) == 3:
            ap = ap.rearrange("p (a b c) -> p a b c", a=shape[0], b=shape[1])
        return ap


def build(debug=(), stop=None):
    nc = bass.Bass("TRN2", target_bir_lowering=False)
    st = ExitStack()

    def din(name, shape, dt=F32):
        return nc.dram_tensor(name, list(shape), dt, kind="ExternalInput").ap()

    def dscr(name, shape, dt):
        kind = "ExternalOutput" if name in debug else "Internal"
        return nc.dram_tensor(name, list(shape), dt, kind=kind).ap()

    xfull = din("xfull", [T, D])
    xq = din("xq", [NOWN, D])
    cT_d = din("cT", [128, 16])
    w_ada = din("w_ada", [D, 6 * D])
    b_adaT_d = din("b_adaT", [128, 96])
    gT_d = din("gT", [128, 64])
    w_in = din("w_in", [D, DIN])
    w_ki2 = din("w_ki2", [D, 128])
    w_mo = din("w_mo", [1024, D])
    w_do = din("w_do", [1024, D])
    w_o = din("w_o", [D, D])
    w_ff1 = din("w_ff1", [D, DFF])
    w_ff2 = din("w_ff2", [DFF, D])
    ropeF = din("ropeF", [4, 128, T])
    ropeQ = din("ropeQ", [4, 128, NOWN])
    cst = din("cst", [128, 5 * 128 + 8 + 32])
    esel_d = din("esel", [128, 16 * 128])
    kpos_d = din("kposB", [128, T])
    out_d = nc.dram_tensor("out", [NOWN, D], F32, kind="ExternalOutput").ap()

    kTa_d = dscr("kTa", [8, 128, T], BF16)
    kTb_d = dscr("kTb", [8, 128, T], BF16)
    va_d = dscr("va", [8, 128, 32, 129], BF16)
    vb_d = dscr("vb", [8, 128, 32, 129], BF16)
    QaT_d = dscr("QaT", [8, 128, NOWN], BF16)
    QbT_d = dscr("QbT", [8, 128, NOWN], BF16)
    QiT_d = dscr("QiT", [8, 128, NOWN], BF16)
    KaTo_d = dscr("KaTo", [8, 128, NOWN], BF16)
    Vao_d = dscr("Vao", [128, 8, 8, 129], BF16)
    hTo_d = dscr("hTo", [128, 16, NOWN], BF16)
    atTa_d = dscr("atTa", [8, 128, NOWN], BF16)
    atTb_d = dscr("atTb", [8, 128, NOWN], BF16)
    gt_d = dscr("gtrow", [2, D], F32)
    x1_d = dscr("x1", [NOWN, D], F32)
    dbg_d = dscr("dbg", [128, 4096], F32)

    arena_t = st.enter_context(nc.sbuf_tensor("sb_arena", [128, ARENA_BYTES // 2], BF16))
    AR = Arena(arena_t, ARENA_BYTES)
    cstF = st.enter_context(nc.sbuf_tensor("sb_cstF", [128, 5 * 128 + 8 + 32], F32))
    cstB = st.enter_context(nc.sbuf_tensor("sb_cstB", [128, 4 * 128], BF16))
    eselB = st.enter_context(nc.sbuf_tensor("sb_eselB", [128, 16 * 128], BF16))
    modT = st.enter_context(nc.sbuf_tensor("sb_modT", [128, 96], F32))
    gT = st.enter_context(nc.sbuf_tensor("sb_gT", [128, 64], F32))
    vecs = st.enter_context(nc.sbuf_tensor("sb_vecs", [128, 6 * 16], F32))
    kiT2 = st.enter_context(nc.sbuf_tensor("sb_kiT2", [128, T], BF16))
    ksum = st.enter_context(nc.sbuf_tensor("sb_ksum", [128, 128], F32))
    kmeanT = st.enter_context(nc.sbuf_tensor("sb_kmeanT", [128, 128], BF16))
    wabs = st.enter_context(nc.sbuf_tensor("sb_wabs", [128, 128], F32))
    wsgn = st.enter_context(nc.sbuf_tensor("sb_wsgn", [128, 128], F32))
    small = st.enter_context(nc.sbuf_tensor("sb_small", [128, 64], F32))
    psb = [st.enter_context(nc.psum_tensor("ps%d" % i, [128, 512], F32)) for i in range(8)]

    identF = cstF[:, 0:128]
    blockend = cstF[:, 512:640]
    qposT = cstF[:, 640:648]
    ftab = cstF[:, 648:680]
    identB = cstB[:, 0:128]
    psw128 = cstB[:, 128:256]
    psw64 = cstB[:, 256:384]
    triB = cstB[:, 384:512]
    G1T, S1T, GT1T = vecs[:, 0:16], vecs[:, 16:32], vecs[:, 32:48]
    G2T, S2T, GT2T = vecs[:, 48:64], vecs[:, 64:80], vecs[:, 80:96]
    neghalf = small[:, 0:1]
    epsc = small[:, 1:2]
    sm_ctr = [2]

    def smcol(n=1):
        if sm_ctr[0] + n > 64:
            sm_ctr[0] = 2
        a = sm_ctr[0]
        sm_ctr[0] += n
        return small[:, a:a + n], ("small", a)

    P = Prog(nc)
    A = P.add

    def PS(i):
        return psb[i][:, :]

    def PSB(i):
        return psb[i][:, :].bitcast(BF16)

    def dma(eng, out, in_, reads, writes, key, nonc=False):
        if nonc:
            return A(eng, lambda e: e.dma_start(out=out, in_=in_, allow_slow_non_contiguous=True),
                     reads=reads, writes=writes, dma_key=key)
        return A(eng, lambda e: e.dma_start(out=out, in_=in_), reads=reads, writes=writes, dma_key=key)

    def mm(out, lhsT, rhs, start, stop, reads, writes):
        return A("pe", lambda e: e.matmul(out, lhsT, rhs, start=start, stop=stop), reads=reads, writes=writes)

    def tr(out, in_, ident, reads, writes):
        return A("pe", lambda e: e.transpose(out, in_, ident), reads=reads, writes=writes)

    dma("sp", cstF[:, :], cst, [], ["cstF"], "c0")
    dma("sp", gT[:, :], gT_d, [], ["gT"], "c2")
    A("dve", lambda e: e.tensor_copy(out=cstB[:, :], in_=cstF[:, 0:512]), reads=["cstF"], writes=["cstB"])
    A("dve", lambda e: e.memset(neghalf, -0.5), writes=["neghalf"])
    A("dve", lambda e: e.memset(epsc, 1e-6), reads=["neghalf"], writes=["neghalf"])
    A("dve", lambda e: e.memset(ksum[:, :], 0.0), writes=["ksum"])

    AR.reset()
    cT = AR.alloc([16], F32)
    csT = AR.alloc([16], BF16)
    badaT = AR.alloc([96], F32)
    eselF = AR.alloc([16 * 128], F32)
    wada = [AR.alloc([16, 512], BF16) for _ in range(3)]
    dma("sp", cT, cT_d, [], ["cT"], "c3")
    dma("sp", badaT, b_adaT_d, [], ["badaT"], "c4")
    dma("sp", eselF, esel_d, [], ["eselF"], "c1")
    A("dve", lambda e: e.tensor_copy(out=eselB[:, :], in_=eselF), reads=["eselF"], writes=["eselB"])
    A("act", lambda e: e.activation(out=csT, in_=cT, func=AF.Silu), reads=["cT"], writes=["csT"])

    def load_wada(c):
        src = w_ada[:, c * 512:(c + 1) * 512].rearrange("(kc p) n -> p kc n", p=128)
        dma("pool", wada[c % 3], src, [], [("wada", c % 3)], ("wada", c % 3))

    load_wada(0)
    load_wada(1)
    for c in range(24):
        if c + 2 < 24:
            load_wada(c + 2)
        wb = wada[c % 3]
        for ci in range(4):
            ct = c * 4 + ci
            for kc in range(16):
                mm(psb[0][:, ct:ct + 1], wb[:, kc, ci * 128:(ci + 1) * 128], csT[:, kc:kc + 1],
                   kc == 0, kc == 15, reads=[("wada", c % 3), "csT"], writes=[("ps", 0)])
    A("dve", lambda e: e.tensor_tensor(out=modT[:, :], in0=psb[0][:, 0:96], in1=badaT, op=ALU.add),
      reads=[("ps", 0), "badaT"], writes=["modT"])
    A("dve", lambda e: e.scalar_tensor_tensor(out=G1T, in0=modT[:, 16:32], scalar=1.0, in1=gT[:, 0:16],
                                               op0=ALU.add, op1=ALU.mult), reads=["modT", "gT"], writes=["vecs"])
    A("dve", lambda e: e.tensor_copy(out=S1T, in_=modT[:, 0:16]), reads=["modT", "vecs"], writes=["vecs"])
    A("dve", lambda e: e.tensor_tensor(out=GT1T, in0=modT[:, 32:48], in1=gT[:, 16:32], op=ALU.mult),
      reads=["modT", "gT", "vecs"], writes=["vecs"])
    A("dve", lambda e: e.scalar_tensor_tensor(out=G2T, in0=modT[:, 64:80], scalar=1.0, in1=gT[:, 32:48],
                                               op0=ALU.add, op1=ALU.mult), reads=["modT", "gT", "vecs"], writes=["vecs"])
    A("dve", lambda e: e.tensor_copy(out=S2T, in_=modT[:, 48:64]), reads=["modT", "vecs"], writes=["vecs"])
    A("dve", lambda e: e.tensor_tensor(out=GT2T, in0=modT[:, 80:96], in1=gT[:, 48:64], op=ALU.mult),
      reads=["modT", "gT", "vecs"], writes=["vecs"])
    dma("sp", gt_d[0].rearrange("(kc p) -> p kc", p=128), GT1T, ["vecs"], ["gt_d"], "c5", nonc=True)
    dma("sp", gt_d[1].rearrange("(kc p) -> p kc", p=128), GT2T, ["vecs"], ["gt_d"], "c5", nonc=True)
    P.barrier()
    if stop == 0:
        return nc, st, P

    class NormCtx:
        pass

    def norm_setup():
        n = NormCtx()
        n.xt = [AR.alloc([D], F32) for _ in range(2)]
        n.xs = [AR.alloc([D], BF16) for _ in range(2)]
        n.junk = AR.alloc([D], BF16)
        n.i = 0
        return n

    def norm_tile(n, src_rows, G, S, dst_fn, dst_res2, pbanks=(0, 1)):
        i = n.i
        n.i += 1
        xt, xs = n.xt[i % 2], n.xs[i % 2]
        rxt, rxs = ("xt", i % 2), ("xs", i % 2)
        dma("sp", xt, src_rows, [], [rxt], rxt)
        ss, rss = smcol()
        vv, rvv = smcol()
        rs, rrs = smcol()
        A("act", lambda e: e.activation(out=n.junk, in_=xt, func=AF.Square, accum_out=ss),
          reads=[rxt], writes=["njunk", rss])
        import os
        KN = int(os.environ.get("KN", "9"))
        if KN < 1:
            return
        A("act", lambda e: e.activation(out=vv, in_=ss, func=AF.Ln, scale=1.0 / D, bias=epsc),
          reads=[rss, "neghalf"], writes=[rvv])
        A("act", lambda e: e.activation(out=rs, in_=vv, func=AF.Exp, scale=-0.5), reads=[rvv], writes=[rrs])
        if KN < 2:
            return
        A("act", lambda e: e.activation(out=xs, in_=xt, func=AF.Copy, scale=rs), reads=[rxt, rrs], writes=[rxs])
        if KN < 3:
            return
        for kc in range(16):
            b = pbanks[kc // 8]
            tr(PSB(b)[:, (kc % 8) * 128:(kc % 8 + 1) * 128], xs[:, kc * 128:(kc + 1) * 128], identB,
               reads=[rxs, "cstB"], writes=[("ps", b)])
        if KN < 4:
            return
        for kc in range(16):
            b = pbanks[kc // 8]
            src = PSB(b)[:, (kc % 8) * 128:(kc % 8 + 1) * 128]
            dst = dst_fn(kc)
            KE = os.environ.get("KE", "mix")
            if (kc // 8 == 0 and KE == "mix") or KE == "dve":
                A("dve", lambda e, src=src, dst=dst, kc=kc: e.tensor_scalar(
                    out=dst, in0=src, scalar1=G[:, kc:kc + 1], scalar2=S[:, kc:kc + 1], op0=ALU.mult, op1=ALU.add),
                  reads=[("ps", b), "vecs"], writes=[dst_res2[0]])
            else:
                A("act", lambda e, src=src, dst=dst, kc=kc: e.activation(
                    out=dst, in_=src, func=AF.Identity, scale=G[:, kc:kc + 1], bias=S[:, kc:kc + 1]),
                  reads=[("ps", b), "vecs"], writes=[dst_res2[1]])

    class WStream:
        def __init__(self, name, nbuf, shape):
            self.name = name
            self.bufs = [AR.alloc(shape, BF16) for _ in range(nbuf)]
            self.n = 0

        def load(self, src_ap, view=None):
            i = self.n % len(self.bufs)
            self.n += 1
            dst = self.bufs[i] if view is None else view(self.bufs[i])
            r = (self.name, i)
            dma("pool", dst, src_ap, [], [r], r)
            return self.bufs[i], r

    def wsrc(w, c0, ncols):
        return w[:, c0:c0 + ncols].rearrange("(kc p) n -> p kc n", p=128)

    class RopeCtx:
        pass

    def rope_setup():
        r = RopeCtx()
        r.xb = [AR.alloc([512], BF16) for _ in range(2)]
        r.t1 = [AR.alloc([512], F32) for _ in range(2)]
        r.t2 = [AR.alloc([512], F32) for _ in range(2)]
        r.i = 0
        return r

    def rope(r, pbank, swbank, cosT, sinT, psw, out_ap, out_res, tab_res):
        i = r.i
        r.i += 1
        xb, t1, t2 = r.xb[i % 2], r.t1[i % 2], r.t2[i % 2]
        import os
        KR = int(os.environ.get("KR", "9"))
        if KR < 1:
            return
        A("act", lambda e: e.activation(out=xb, in_=PS(pbank), func=AF.Copy), reads=[("ps", pbank)], writes=[("rxb", i % 2)])
        if KR < 2:
            return
        mm(PS(swbank), psw, xb, True, True, reads=[("rxb", i % 2), "cstB"], writes=[("ps", swbank)])
        if KR < 3:
            return
        A("dve", lambda e: e.tensor_tensor(out=t1, in0=PS(pbank), in1=cosT, op=ALU.mult),
          reads=[("ps", pbank), tab_res], writes=[("rt1", i % 2)])
        A("dve", lambda e: e.tensor_tensor(out=t2, in0=PS(swbank), in1=sinT, op=ALU.mult),
          reads=[("ps", swbank), tab_res], writes=[("rt2", i % 2)])
        if KR < 4:
            return
        A("dve", lambda e: e.tensor_tensor(out=out_ap, in0=t1, in1=t2, op=ALU.add),
          reads=[("rt1", i % 2), ("rt2", i % 2)], writes=[out_res])

    AR.reset()
    nctx = norm_setup()
    rctx = rope_setup()
    hT = AR.alloc([16, 1024], BF16)
    ws = WStream("w1", 3, [16, 512])
    tabs = AR.alloc([4, 1024], F32)
    kst = [AR.alloc([1024], BF16) for _ in range(2)]
    vst = [AR.alloc([4, 129], BF16) for _ in range(3)]
    for i in range(3):
        A("dve", lambda e, i=i: e.memset(vst[i][:, :, 128:129], 1.0), writes=[("vst", i)])
    kcnt = [0]
    vcnt = [0]
    pk = [0]
    for tg in range(4):
        t0 = tg * 1024
        dma("sp", tabs, ropeF[:, :, t0:t0 + 1024].rearrange("f p t -> p f t"), [], ["tabs"], "tabs")
        for tt in range(8):
            norm_tile(nctx, xfull[t0 + tt * 128:t0 + (tt + 1) * 128, :], G1T, S1T,
                      (lambda kc, tt=tt: hT[:, kc, tt * 128:(tt + 1) * 128]), (("hT", tt, 0), ("hT", tt, 1)))
        hT_res = [("hT", tt, q) for tt in range(8) for q in range(2)]
        if stop == 1:
            P.barrier()
            return nc, st, P
        kjobs = []
        kjobs.append((wsrc(w_in, C_KA, 512), 512, [(ci, "a", ci) for ci in range(4)]))
        kjobs.append((wsrc(w_in, C_KA + 512, 512), 512, [(ci, "a", 4 + ci) for ci in range(4)]))
        kjobs.append((wsrc(w_in, C_KB, 512), 512, [(ci, "b", ci) for ci in range(4)]))
        kjobs.append((wsrc(w_in, C_KB + 512, 512), 512, [(ci, "b", 4 + ci) for ci in range(4)]))
        kjobs.append((wsrc(w_ki2, 0, 128), 128, [(0, "i", 0)]))
        for (src, ncols, tiles) in kjobs:
            wb, wr = ws.load(src, view=(lambda b, ncols=ncols: b[:, :, 0:ncols]))
            for (ci, kind, head) in tiles:
                ks = kst[kcnt[0] % 2]
                ksr = ("kst", kcnt[0] % 2)
                kcnt[0] += 1
                for half in range(2):
                    pb = 2 + (pk[0] % 2)
                    sb_ = 4 + (pk[0] % 2)
                    pk[0] += 1
                    for kc in range(16):
                        mm(PS(pb), wb[:, kc, ci * 128:(ci + 1) * 128], hT[:, kc, half * 512:(half + 1) * 512],
                           kc == 0, kc == 15, reads=[wr] + hT_res[half * 8:half * 8 + 8], writes=[("ps", pb)])
                    if kind == "i":
                        cosT, sinT, psw = tabs[:, 2, half * 512:(half + 1) * 512], tabs[:, 3, half * 512:(half + 1) * 512], psw64
                        oap, ores = kiT2[:, t0 + half * 512:t0 + (half + 1) * 512], "kiT2"
                    else:
                        cosT, sinT, psw = tabs[:, 0, half * 512:(half + 1) * 512], tabs[:, 1, half * 512:(half + 1) * 512], psw128
                        oap, ores = ks[:, half * 512:(half + 1) * 512], ksr
                    rope(rctx, pb, sb_, cosT, sinT, psw, oap, ores, "tabs")
                if kind == "a":
                    A("dve", lambda e, ks=ks, head=head, tg=tg: e.tensor_reduce(
                        out=ksum[:, head * 16 + tg * 4:head * 16 + tg * 4 + 4],
                        in_=ks.rearrange("p (n j) -> p n j", j=256), axis=AX.X, op=ALU.add),
                      reads=[ksr], writes=["ksum"])
                if kind in ("a", "b"):
                    dst = (kTa_d if kind == "a" else kTb_d)[head, :, t0:t0 + 1024]
                    dma("sp", dst, ks, [ksr], [("kT", kind, head)], ("kstore", kcnt[0] % 2))
        if stop == 2:
            P.barrier()
            return nc, st, P
        for (c0, vd, hb) in ((C_VA, va_d, 0), (C_VA + 512, va_d, 4), (C_VB, vb_d, 0), (C_VB + 512, vb_d, 4)):
            wb, wr = ws.load(wsrc(w_in, c0, 512))
            for tt in range(8):
                pb = 6 + (vcnt[0] % 2)
                vs = vst[vcnt[0] % 3]
                vsr = ("vst", vcnt[0] % 3)
                vcnt[0] += 1
                for kc in range(16):
                    mm(PS(pb), hT[:, kc, tt * 128:(tt + 1) * 128], wb[:, kc, :], kc == 0, kc == 15,
                       reads=[wr, ("hT", tt, 0), ("hT", tt, 1)], writes=[("ps", pb)])
                A("act", lambda e, vs=vs, pb=pb: e.activation(
                    out=vs[:, :, 0:128], in_=PS(pb).rearrange("p (h d) -> p h d", h=4), func=AF.Copy),
                  reads=[("ps", pb)], writes=[vsr])
                gtile = tg * 8 + tt
                dma("sp", vd[hb:hb + 4, :, gtile, :].rearrange("h p c -> p h c"), vs, [vsr],
                    [("vd", id(vd), hb, gtile)], ("vstore", vcnt[0] % 3))
        if stop == 3:
            P.barrier()
            return nc, st, P
    A("dve", lambda e: e.tensor_scalar(out=kmeanT[:, :], in0=ksum[:, :], scalar1=1.0 / 256.0, scalar2=None, op0=ALU.mult),
      reads=["ksum"], writes=["kmeanT"])
    P.barrier()
    if stop == 4:
        return nc, st, P

    AR.reset()
    nctx = norm_setup()
    rctx = rope_setup()
    hT = AR.alloc([16, 1024], BF16)
    ws = WStream("w2", 3, [16, 512])
    tabs = AR.alloc([4, 1024], F32)
    kst = [AR.alloc([1024], BF16) for _ in range(2)]
    vst = [AR.alloc([4, 129], BF16) for _ in range(3)]
    wwi = AR.alloc([16, 16], BF16)
    for i in range(3):
        A("dve", lambda e, i=i: e.memset(vst[i][:, :, 128:129], 1.0), writes=[("vst", i)])
    dma("sp", tabs, ropeQ.rearrange("f p t -> p f t"), [], ["tabs"], "tabs")
    dma("pool", wwi, w_in[:, C_WI:C_WI + 16].rearrange("(kc p) n -> p kc n", p=128), [], ["wwi"], "wwi")
    for tt in range(8):
        norm_tile(nctx, xq[tt * 128:(tt + 1) * 128, :], G1T, S1T,
                  (lambda kc, tt=tt: hT[:, kc, tt * 128:(tt + 1) * 128]), (("hT", tt, 0), ("hT", tt, 1)))
    hT_res = [("hT", tt, q) for tt in range(8) for q in range(2)]
    dma("sp", hTo_d, hT, hT_res, ["hTo_d"], "hTo")
    kcnt = [0]
    pk = [0]
    for (c0, dst_d, r64) in ((C_QA, QaT_d, False), (C_KA, KaTo_d, False), (C_QB, QbT_d, False), (C_QI, QiT_d, True)):
        for ch in range(2):
            wb, wr = ws.load(wsrc(w_in, c0 + ch * 512, 512))
            for ci in range(4):
                head = ch * 4 + ci
                ks = kst[kcnt[0] % 2]
                ksr = ("kst", kcnt[0] % 2)
                kcnt[0] += 1
                for half in range(2):
                    pb = 2 + (pk[0] % 2)
                    sb_ = 4 + (pk[0] % 2)
                    pk[0] += 1
                    for kc in range(16):
                        mm(PS(pb), wb[:, kc, ci * 128:(ci + 1) * 128], hT[:, kc, half * 512:(half + 1) * 512],
                           kc == 0, kc == 15, reads=[wr] + hT_res[half * 8:half * 8 + 8], writes=[("ps", pb)])
                    sl = slice(half * 512, (half + 1) * 512)
                    if r64:
                        rope(rctx, pb, sb_, tabs[:, 2, sl], tabs[:, 3, sl], psw64, ks[:, sl], ksr, "tabs")
                    else:
                        rope(rctx, pb, sb_, tabs[:, 0, sl], tabs[:, 1, sl], psw128, ks[:, sl], ksr, "tabs")
                dma("sp", dst_d[head, :, :], ks, [ksr], [("qd", c0, head)], ("kstore", kcnt[0] % 2))
    vcnt = [0]
    for ch in range(2):
        wb, wr = ws.load(wsrc(w_in, C_VA + ch * 512, 512))
        for tt in range(8):
            pb = 6 + (vcnt[0] % 2)
            vs = vst[vcnt[0] % 3]
            vsr = ("vst", vcnt[0] % 3)
            vcnt[0] += 1
            for kc in range(16):
                mm(PS(pb), hT[:, kc, tt * 128:(tt + 1) * 128], wb[:, kc, :], kc == 0, kc == 15,
                   reads=[wr, ("hT", tt, 0), ("hT", tt, 1)], writes=[("ps", pb)])
            A("act", lambda e, vs=vs, pb=pb: e.activation(
                out=vs[:, :, 0:128], in_=PS(pb).rearrange("p (h d) -> p h d", h=4), func=AF.Copy),
              reads=[("ps", pb)], writes=[vsr])
            dma("sp", Vao_d[:, tt, ch * 4:ch * 4 + 4, :], vs, [vsr], [("vao", tt, ch)], ("vstore", vcnt[0] % 3))
    for tt in range(8):
        pb = 6 + (tt % 2)
        for kc in range(16):
            mm(psb[pb][:, 0:16], hT[:, kc, tt * 128:(tt + 1) * 128], wwi[:, kc, :], kc == 0, kc == 15,
               reads=["wwi", ("hT", tt, 0), ("hT", tt, 1)], writes=[("ps", pb)])
        A("act", lambda e, tt=tt, pb=pb: e.activation(out=wabs[:, tt * 16:(tt + 1) * 16], in_=psb[pb][:, 0:16], func=AF.Abs),
          reads=[("ps", pb)], writes=["wabs"])
        A("act", lambda e, tt=tt, pb=pb: e.activation(out=wsgn[:, tt * 16:(tt + 1) * 16], in_=psb[pb][:, 0:16], func=AF.Sign),
          reads=[("ps", pb)], writes=["wsgn"])
    P.barrier()
    if stop == 5:
        return nc, st, P

    def finalize_heads(h, g, asb, stg, dst_d, ia):
        for qs in range(4):
            rd, rrd = smcol()
            A("dve", lambda e, rd=rd, qs=qs: e.reciprocal(out=rd, in_=psb[2 + qs][:, 128:129]),
              reads=[("ps", 2 + qs)], writes=[rrd])
            A("act", lambda e, rd=rd, qs=qs: e.activation(out=asb[:, qs, :], in_=psb[2 + qs][:, 0:128], func=AF.Copy, scale=rd),
              reads=[("ps", 2 + qs), rrd], writes=[("asb", ia, qs)])
        for qs in range(4):
            tr(PSB(7)[:, qs * 128:(qs + 1) * 128], asb[:, qs, :], identB, reads=[("asb", ia, qs), "cstB"], writes=[("ps", 7)])
        A("dve", lambda e: e.tensor_copy(out=stg, in_=PSB(7)[:, 0:512]), reads=[("ps", 7)], writes=[("stg", ia)])
        dma("sp", dst_d[h, :, g * 512:(g + 1) * 512], stg, [("stg", ia)], [("atd", id(dst_d), h, g)], ("atst", ia))

    for g in range(2):
        NKT = 16 if g == 0 else 32
        NK = NKT * 128
        AR.reset()
        QiG = AR.alloc([8, 512], BF16)
        kposB = AR.alloc([NK], F32)
        scores = [AR.alloc([NK], F32) for _ in range(2)]
        junks = [AR.alloc([NK], BF16) for _ in range(2)]
        m01s = [AR.alloc([NK], BF16) for _ in range(2)]
        maskT = AR.alloc([NKT, 512], BF16)
        bsts = [AR.alloc([64], F32) for _ in range(2)]
        QbG = AR.alloc([8, 512], BF16)
        Kh = [AR.alloc([NK], BF16) for _ in range(2)]
        Vh = [AR.alloc([NKT, 129], BF16) for _ in range(2)]
        esb = [AR.alloc([512], BF16) for _ in range(2)]
        pT = [AR.alloc([512], BF16) for _ in range(2)]
        asb = [AR.alloc([4, 128], BF16) for _ in range(2)]
        stg = [AR.alloc([512], BF16) for _ in range(2)]
        dma("sp", QiG, QiT_d[:, :, g * 512:(g + 1) * 512].rearrange("h p t -> p h t"), [], ["QiG"], "QiG")
        dma("sp", kposB, kpos_d[:, 0:NK], [], ["kposB"], "kposB")
        dma("sp", QbG, QbT_d[:, :, g * 512:(g + 1) * 512].rearrange("h p t -> p h t"), [], ["QbG"], "QbG")
        cnt = [0]
        NB = N_BISECT
        for qp in range(2):
            for z in range(2):
                qt = qp * 2 + z
                Tq = 4 * g + qt
                score = scores[z]
                for c in range(NKT // 4):
                    scr = ("score", z, c)
                    sc = score[:, c * 512:(c + 1) * 512]
                    for h in range(16):
                        half, pair = h % 2, h // 2
                        bs_, br_ = cnt[0] % 2, 2 + (cnt[0] % 2)
                        cnt[0] += 1
                        mm(PS(bs_), QiG[64 * half:64 * half + 64, pair, qt * 128:(qt + 1) * 128],
                           kiT2[64 * half:64 * half + 64, c * 512:(c + 1) * 512], True, True,
                           reads=["QiG", "kiT2"], writes=[("ps", bs_)])
                        col = Tq * 16 + h
                        A("act", lambda e, bs_=bs_, br_=br_, col=col: e.activation(
                            out=PS(br_), in_=PS(bs_), func=AF.Relu, scale=wabs[:, col:col + 1]),
                          reads=[("ps", bs_), "wabs"], writes=[("ps", br_)])
                        if h == 0:
                            A("dve", lambda e, br_=br_, col=col, sc=sc: e.tensor_scalar(
                                out=sc, in0=PS(br_), scalar1=wsgn[:, col:col + 1], scalar2=None, op0=ALU.mult),
                              reads=[("ps", br_), "wsgn"], writes=[scr])
                        else:
                            A("dve", lambda e, br_=br_, col=col, sc=sc: e.scalar_tensor_tensor(
                                out=sc, in0=PS(br_), scalar=wsgn[:, col:col + 1], in1=sc, op0=ALU.mult, op1=ALU.add),
                              reads=[("ps", br_), "wsgn", scr], writes=[scr])
            for z in range(2):
                qt = qp * 2 + z
                Tq = 4 * g + qt
                score, junk, bst = scores[z], junks[z], bsts[z]
                allsc = [("score", z, c) for c in range(NKT // 4)]
                rmx, rmn, w0, mid, cntc, uu, lo = [bst[:, i:i + 1] for i in range(7)]
                tab = bst[:, 8:8 + NB + 1]
                B_ = lambda i, z=z: ("bst", z, i)
                A("dve", lambda e: e.tensor_reduce(out=rmx, in_=score, axis=AX.X, op=ALU.max), reads=allsc, writes=[B_(0)])
                A("dve", lambda e: e.tensor_reduce(out=rmn, in_=score, axis=AX.X, op=ALU.min), reads=allsc, writes=[B_(1)])
                A("dve", lambda e: e.tensor_tensor(out=w0, in0=rmx, in1=rmn, op=ALU.subtract), reads=[B_(0), B_(1)], writes=[B_(2)])
                A("dve", lambda e: e.tensor_scalar(out=w0, in0=w0, scalar1=1.001, scalar2=1e-20, op0=ALU.mult, op1=ALU.add),
                  reads=[B_(2)], writes=[B_(2)])
                A("dve", lambda e: e.tensor_scalar(out=tab, in0=ftab[:, 0:NB + 1], scalar1=w0, scalar2=None, op0=ALU.mult),
                  reads=[B_(2), "cstF"], writes=[B_(8)])
                if z == 0:
                    A("dve", lambda e: e.tensor_tensor(out=mid, in0=rmn, in1=tab[:, 0:1], op=ALU.add), reads=[B_(1), B_(8)], writes=[B_(3)])
                else:
                    A("dve", lambda e: e.tensor_scalar(out=mid, in0=rmn, scalar1=tab[:, 0:1], scalar2=-1.0, op0=ALU.add, op1=ALU.mult),
                      reads=[B_(1), B_(8)], writes=[B_(3)])
                A("dve", lambda e, Tq=Tq: e.tensor_scalar(out=junk, in0=kposB, scalar1=qposT[:, Tq:Tq + 1], scalar2=None,
                                                          op0=ALU.is_gt), reads=["kposB", "cstF"], writes=[("junk", z)])
                A("dve", lambda e: e.scalar_tensor_tensor(out=score, in0=junk, scalar=-1.0e9, in1=score, op0=ALU.mult, op1=ALU.add),
                  reads=allsc + [("junk", z)], writes=allsc)
            for it in range(1, NB + 1):
                for z in range(2):
                    score, junk, bst = scores[z], junks[z], bsts[z]
                    allsc = [("score", z, c) for c in range(NKT // 4)]
                    rmx, rmn, w0, mid, cntc, uu, lo = [bst[:, i:i + 1] for i in range(7)]
                    tab = bst[:, 8:8 + NB + 1]
                    B_ = lambda i, z=z: ("bst", z, i)
                    if z == 0:
                        A("dve", lambda e: e.tensor_scalar(out=junk, in0=score, scalar1=mid, scalar2=None, op0=ALU.is_ge, op1=ALU.add,
                                                           accum_out=cntc), reads=allsc + [B_(3)], writes=[("junk", z), B_(4)])
                        A("dve", lambda e, it=it: e.tensor_scalar(out=uu, in0=cntc, scalar1=255.5, scalar2=tab[:, it - 1:it],
                                                                 op0=ALU.is_ge, op1=ALU.mult), reads=[B_(4), B_(8)], writes=[B_(5)])
                        A("dve", lambda e, it=it: e.tensor_scalar(out=mid, in0=mid, scalar1=tab[:, it:it + 1], scalar2=uu,
                                                                 op0=ALU.subtract, op1=ALU.add), reads=[B_(3), B_(5), B_(8)], writes=[B_(3)])
                    else:
                        A("act", lambda e: e.activation(out=junk, in_=score, func=AF.Sign, bias=mid, scale=1.0, accum_out=cntc),
                          reads=allsc + [B_(3)], writes=[("junk", z), B_(4)])
                        A("dve", lambda e, it=it: e.tensor_scalar(out=uu, in0=cntc, scalar1=float(511 - NK), scalar2=tab[:, it - 1:it],
                                                                 op0=ALU.is_ge, op1=ALU.mult), reads=[B_(4), B_(8)], writes=[B_(5)])
                        A("dve", lambda e, it=it: e.tensor_scalar(out=mid, in0=mid, scalar1=tab[:, it:it + 1], scalar2=uu,
                                                                 op0=ALU.add, op1=ALU.subtract), reads=[B_(3), B_(5), B_(8)], writes=[B_(3)])
            for z in range(2):
                qt = qp * 2 + z
                score, junk, bst, m01 = scores[z], junks[z], bsts[z], m01s[z]
                allsc = [("score", z, c) for c in range(NKT // 4)]
                rmx, rmn, w0, mid, cntc, uu, lo = [bst[:, i:i + 1] for i in range(7)]
                tab = bst[:, 8:8 + NB + 1]
                B_ = lambda i, z=z: ("bst", z, i)
                if z == 0:
                    A("dve", lambda e: e.tensor_tensor(out=lo, in0=mid, in1=tab[:, NB:NB + 1], op=ALU.subtract),
                      reads=[B_(3), B_(8)], writes=[B_(6)])
                else:
                    A("dve", lambda e: e.tensor_scalar(out=lo, in0=mid, scalar1=-1.0, scalar2=tab[:, NB:NB + 1], op0=ALU.mult, op1=ALU.subtract),
                      reads=[B_(3), B_(8)], writes=[B_(6)])
                A("dve", lambda e: e.tensor_scalar(out=m01, in0=score, scalar1=lo, scalar2=None, op0=ALU.is_ge),
                  reads=allsc + [B_(6)], writes=[("m01", z)])
                for k8 in range(NKT // 8):
                    bank = 4 + (k8 % 2)
                    for j in range(8):
                        kt = k8 * 8 + j
                        tr(PSB(bank)[:, j * 128:(j + 1) * 128], m01[:, kt * 128:(kt + 1) * 128], identB,
                           reads=[("m01", z), "cstB"], writes=[("ps", bank)])
                    A("act", lambda e, bank=bank, k8=k8, qt=qt: e.activation(
                        out=maskT[:, k8 * 8:(k8 + 1) * 8, qt * 128:(qt + 1) * 128],
                        in_=PSB(bank).rearrange("p (j q) -> p j q", j=8), func=AF.Copy),
                      reads=[("ps", bank)], writes=[("maskT", qt)])
        maskT_res = [("maskT", qt) for qt in range(4)]
        if stop == 6:
            dma("sp", dbg_d[:, 0:NKT * 256].bitcast(BF16) if False else x1_d[0:128, :].bitcast(BF16)[:, 0:4096].rearrange("p (a b) -> p a b", a=8)[:, :, :],
                maskT[:, 0:8, :], maskT_res, ["dbgm"], "dbgm") if False else None
            P.barrier()
            return nc, st, P

        if stop == 61:
            P.barrier()
            continue
        def loadKV(h, kd, vd, pref):
            i = h % 2
            dma("sp", Kh[i], kd[h, :, 0:NK], [], [(pref + "K", i)], (pref + "K", i))
            dma("sp", Vh[i], vd[h, :, 0:NKT, :], [], [(pref + "V", i)], (pref + "V", i))

        loadKV(0, kTb_d, vb_d, "b")
        for h in range(8):
            if h + 1 < 8:
                loadKV(h + 1, kTb_d, vb_d, "b")
            i = h % 2
            for kt in range(NKT):
                bs_ = kt % 2
                mm(PS(bs_), Kh[i][:, kt * 128:(kt + 1) * 128], QbG[:, h, :], True, True,
                   reads=[("bK", i), "QbG"], writes=[("ps", bs_)])
                A("act", lambda e, bs_=bs_, kt=kt: e.activation(out=esb[kt % 2], in_=PS(bs_), func=AF.Exp, scale=SCALE),
                  reads=[("ps", bs_)], writes=[("esb", kt % 2)])
                A("dve", lambda e, kt=kt: e.tensor_tensor(out=pT[kt % 2], in0=esb[kt % 2], in1=maskT[:, kt, :], op=ALU.mult),
                  reads=[("esb", kt % 2)] + maskT_res, writes=[("pT", kt % 2)])
                for qs in range(4):
                    mm(psb[2 + qs][:, 0:129], pT[kt % 2][:, qs * 128:(qs + 1) * 128], Vh[i][:, kt, :], kt == 0, kt == NKT - 1,
                       reads=[("pT", kt % 2), ("bV", i)], writes=[("ps", 2 + qs)])
            finalize_heads(h, g, asb[h % 2], stg[h % 2], atTb_d, h % 2)
        P.barrier()
    if stop == 7:
        return nc, st, P

    AR.reset()
    QaT = AR.alloc([8, 1024], BF16)
    KaTo = AR.alloc([8, 1024], BF16)
    Vao = AR.alloc([8, 8, 129], BF16)
    MT = AR.alloc([8, 1024], BF16)
    gsc = AR.alloc([6, 128], F32)
    m8 = AR.alloc([64], F32)
    Mq = AR.alloc([128], BF16)
    Kh = [AR.alloc([4096], BF16) for _ in range(2)]
    Vh = [AR.alloc([32, 129], BF16) for _ in range(2)]
    pT = [AR.alloc([512], BF16) for _ in range(2)]
    eo = [AR.alloc([128], BF16) for _ in range(2)]
    asb = [AR.alloc([4, 128], BF16) for _ in range(2)]
    stg = [AR.alloc([512], BF16) for _ in range(2)]
    dma("sp", QaT, QaT_d.rearrange("h p t -> p h t"), [], ["QaT"], "QaT")
    dma("sp", KaTo, KaTo_d.rearrange("h p t -> p h t"), [], ["KaTo"], "KaTo")
    dma("sp", Vao, Vao_d, [], ["Vao"], "Vao")
    pastm, negm, gm, sel = gsc[:, 0, :], gsc[:, 1, :], gsc[:, 2, :], gsc[:, 3, :]
    A("dve", lambda e: e.memset(MT, 0.0), writes=[("MT", q) for q in range(8)])
    for Tq in range(8):
        for h in range(8):
            mm(psb[7][:, h * 16:(h + 1) * 16], QaT[:, h, Tq * 128:(Tq + 1) * 128], kmeanT[:, h * 16:(h + 1) * 16], True, True,
               reads=["QaT", "kmeanT"], writes=[("ps", 7)])
        A("dve", lambda e, Tq=Tq: e.tensor_scalar(out=pastm, in0=blockend, scalar1=qposT[:, Tq:Tq + 1], scalar2=None, op0=ALU.is_le),
          reads=["cstF"], writes=["pastm"])
        A("dve", lambda e: e.tensor_scalar(out=negm, in0=pastm, scalar1=1.0, scalar2=BIGM, op0=ALU.subtract, op1=ALU.mult),
          reads=["pastm"], writes=["negm"])
        A("dve", lambda e: e.tensor_tensor(out=gm, in0=psb[7][:, 0:128], in1=negm, op=ALU.add),
          reads=[("ps", 7), "negm"], writes=["gm"])
        for h in range(8):
            A("dve", lambda e, h=h: e.max(out=m8[:, h * 8:(h + 1) * 8], in_=gm[:, h * 16:(h + 1) * 16]),
              reads=["gm"], writes=[("m8", h)])
        for h in range(8):
            A("dve", lambda e, h=h: e.tensor_scalar(out=sel[:, h * 16:(h + 1) * 16], in0=gm[:, h * 16:(h + 1) * 16],
                                                     scalar1=m8[:, h * 8 + 2:h * 8 + 3], scalar2=None, op0=ALU.is_ge),
              reads=["gm", ("m8", h)], writes=[("sel", h)])
        A("dve", lambda e: e.tensor_tensor(out=sel, in0=sel, in1=pastm, op=ALU.mult),
          reads=[("sel", h) for h in range(8)] + ["pastm"], writes=[("sel", h) for h in range(8)])
        A("dve", lambda e: e.tensor_scalar(out=Mq, in0=sel, scalar1=1.0, scalar2=BIGM, op0=ALU.subtract, op1=ALU.mult),
          reads=[("sel", h) for h in range(8)], writes=["Mq"])
        for h in range(8):
            tr(PSB(6)[0:16, h * 128:(h + 1) * 128], Mq[:, h * 16:(h + 1) * 16], identB, reads=["Mq", "cstB"], writes=[("ps", 6)])
        A("act", lambda e, Tq=Tq: e.activation(out=MT[0:16, :, Tq * 128:(Tq + 1) * 128],
                                               in_=PSB(6)[0:16, :].rearrange("p (h q) -> p h q", h=8), func=AF.Copy),
          reads=[("ps", 6)], writes=[("MT", Tq)])
    for g in range(2):
        NKT = 16 if g == 0 else 32
        NK = NKT * 128
        MT_res = [("MT", 4 * g + q) for q in range(4)]

        def loadKVa(h):
            i = h % 2
            dma("sp", Kh[i][:, 0:NK], kTa_d[h, :, 0:NK], [], [("aK", i)], ("aK", i))
            dma("sp", Vh[i][:, 0:NKT, :], va_d[h, :, 0:NKT, :], [], [("aV", i)], ("aV", i))

        loadKVa(0)
        ecnt = [0]
        for h in range(8):
            if h + 1 < 8:
                loadKVa(h + 1)
            i = h % 2
            for kt in range(NKT):
                bs_ = kt % 2
                mm(PS(bs_), Kh[i][:, kt * 128:(kt + 1) * 128], QaT[:, h, g * 512:(g + 1) * 512], True, False,
                   reads=[("aK", i), "QaT"], writes=[("ps", bs_)])
                n0 = kt // 2
                mm(PS(bs_), eselB[:, n0 * 128:(n0 + 1) * 128], MT[:, h, g * 512:(g + 1) * 512], False, True,
                   reads=["eselB"] + MT_res, writes=[("ps", bs_)])
                A("act", lambda e, bs_=bs_, kt=kt: e.activation(out=pT[kt % 2], in_=PS(bs_), func=AF.Exp, scale=SCALE),
                  reads=[("ps", bs_)], writes=[("pT", kt % 2)])
                for qs in range(4):
                    mm(psb[2 + qs][:, 0:129], pT[kt % 2][:, qs * 128:(qs + 1) * 128], Vh[i][:, kt, :], kt == 0, False,
                       reads=[("pT", kt % 2), ("aV", i)], writes=[("ps", 2 + qs)])
            for qs in range(4):
                Tq = 4 * g + qs
                tiles = [(Tq, True)] if Tq % 2 == 0 else [(Tq - 1, False), (Tq, True)]
                for ti, (ot, diag) in enumerate(tiles):
                    ei = ecnt[0] % 2
                    ecnt[0] += 1
                    mm(psb[6][:, 0:128], KaTo[:, h, ot * 128:(ot + 1) * 128], QaT[:, h, Tq * 128:(Tq + 1) * 128], True, True,
                       reads=["KaTo", "QaT"], writes=[("ps", 6)])
                    A("act", lambda e, ei=ei: e.activation(out=eo[ei], in_=psb[6][:, 0:128], func=AF.Exp, scale=SCALE),
                      reads=[("ps", 6)], writes=[("eo", ei)])
                    if diag:
                        A("dve", lambda e, ei=ei: e.tensor_tensor(out=eo[ei], in0=eo[ei], in1=triB, op=ALU.mult),
                          reads=[("eo", ei), "cstB"], writes=[("eo", ei)])
                    mm(psb[2 + qs][:, 0:129], eo[ei], Vao[:, ot, h, :], False, ti == len(tiles) - 1,
                       reads=[("eo", ei), "Vao"], writes=[("ps", 2 + qs)])
            finalize_heads(h, g, asb[h % 2], stg[h % 2], atTa_d, h % 2)
    P.barrier()
    if stop == 8:
        return nc, st, P

    def rs_from_ss(ss, rss):
        vv, rvv = smcol()
        rs, rrs = smcol()
        A("act", lambda e: e.activation(out=vv, in_=ss, func=AF.Ln, scale=1.0 / D, bias=epsc), reads=[rss, "neghalf"], writes=[rvv])
        A("act", lambda e: e.activation(out=rs, in_=vv, func=AF.Exp, scale=-0.5), reads=[rvv], writes=[rrs])
        return rs, rrs

    AR.reset()
    atA = AR.alloc([8, 512], BF16)
    atB = AR.alloc([8, 512], BF16)
    hTo = AR.alloc([16, 512], BF16)
    ycT = AR.alloc([16, 512], BF16)
    wso = WStream("wso", 2, [8, 512])
    wsg = WStream("wsg", 2, [16, 512])
    sg = [AR.alloc([512], F32) for _ in range(4)]
    ysb = AR.alloc([4, 2048], F32)
    GTB = AR.alloc([2048], F32)
    xt6 = [AR.alloc([2048], F32) for _ in range(2)]
    junk6 = AR.alloc([2048], BF16)
    dma("sp", GTB, gt_d[0].partition_broadcast(128), [], ["GTB"], "GTB")
    for g in range(2):
        gs = slice(g * 512, (g + 1) * 512)
        dma("sp", atA, atTa_d[:, :, gs].rearrange("h p t -> p h t"), [], ["atA"], "atA")
        dma("sp", atB, atTb_d[:, :, gs].rearrange("h p t -> p h t"), [], ["atB"], "atB")
        dma("sp", hTo, hTo_d[:, :, gs], [], ["hTo"], "hTo")
        for cc in range(4):
            wmo, rmo = wso.load(w_mo[:, cc * 512:(cc + 1) * 512].rearrange("(h p) n -> p h n", p=128))
            wdo, rdo = wso.load(w_do[:, cc * 512:(cc + 1) * 512].rearrange("(h p) n -> p h n", p=128))
            wga, rga = wsg.load(wsrc(w_in, C_GA + cc * 512, 512))
            wgb, rgb = wsg.load(wsrc(w_in, C_GB + cc * 512, 512))
            for ci in range(4):
                ct = cc * 4 + ci
                b0 = 4 * (ct % 2)
                cs_ = slice(ci * 128, (ci + 1) * 128)
                for h in range(8):
                    mm(PS(b0), wmo[:, h, cs_], atA[:, h, :], h == 0, h == 7, reads=[rmo, "atA"], writes=[("ps", b0)])
                for h in range(8):
                    mm(PS(b0 + 1), wdo[:, h, cs_], atB[:, h, :], h == 0, h == 7, reads=[rdo, "atB"], writes=[("ps", b0 + 1)])
                for kc in range(16):
                    mm(PS(b0 + 2), wga[:, kc, cs_], hTo[:, kc, :], kc == 0, kc == 15, reads=[rga, "hTo"], writes=[("ps", b0 + 2)])
                for kc in range(16):
                    mm(PS(b0 + 3), wgb[:, kc, cs_], hTo[:, kc, :], kc == 0, kc == 15, reads=[rgb, "hTo"], writes=[("ps", b0 + 3)])
                s0, s1 = sg[2 * (ct % 2)], sg[2 * (ct % 2) + 1]
                r0, r1 = ("sg", 2 * (ct % 2)), ("sg", 2 * (ct % 2) + 1)
                A("act", lambda e, b0=b0, s0=s0: e.activation(out=s0, in_=PS(b0 + 2), func=AF.Sigmoid), reads=[("ps", b0 + 2)], writes=[r0])
                A("act", lambda e, b0=b0, s1=s1: e.activation(out=s1, in_=PS(b0 + 3), func=AF.Sigmoid), reads=[("ps", b0 + 3)], writes=[r1])
                A("dve", lambda e, b0=b0, s0=s0: e.tensor_tensor(out=s0, in0=PS(b0), in1=s0, op=ALU.mult), reads=[("ps", b0), r0], writes=[r0])
                A("dve", lambda e, b0=b0, s1=s1: e.tensor_tensor(out=s1, in0=PS(b0 + 1), in1=s1, op=ALU.mult), reads=[("ps", b0 + 1), r1], writes=[r1])
                A("dve", lambda e, ct=ct, s0=s0, s1=s1: e.tensor_tensor(out=ycT[:, ct, :], in0=s0, in1=s1, op=ALU.add),
                  reads=[r0, r1], writes=[("ycT", ct)])
        yc_res = [("ycT", ct) for ct in range(16)]
        ycnt = [0]
        for cc in range(4):
            wo_, rwo = wsg.load(wsrc(w_o, cc * 512, 512))
            for tt in range(4):
                bank = ycnt[0] % 2
                ycnt[0] += 1
                for kc in range(16):
                    mm(PS(bank), ycT[:, kc, tt * 128:(tt + 1) * 128], wo_[:, kc, :], kc == 0, kc == 15,
                       reads=[rwo] + yc_res, writes=[("ps", bank)])
                A("act", lambda e, bank=bank, tt=tt, cc=cc: e.activation(out=ysb[:, tt, cc * 512:(cc + 1) * 512], in_=PS(bank), func=AF.Copy),
                  reads=[("ps", bank)], writes=[("ysb", tt)])
        for tt in range(4):
            row0 = g * 512 + tt * 128
            xt = xt6[tt % 2]
            rx = ("xt6", tt % 2)
            dma("sp", xt, xq[row0:row0 + 128, :], [], [rx], rx)
            ss, rss = smcol()
            A("act", lambda e, tt=tt, ss=ss: e.activation(out=junk6, in_=ysb[:, tt, :], func=AF.Square, accum_out=ss),
              reads=[("ysb", tt)], writes=["junk6", rss])
            rs, rrs = rs_from_ss(ss, rss)
            A("dve", lambda e, tt=tt, rs=rs: e.scalar_tensor_tensor(out=ysb[:, tt, :], in0=ysb[:, tt, :], scalar=rs, in1=GTB,
                                                                     op0=ALU.mult, op1=ALU.mult),
              reads=[("ysb", tt), rrs, "GTB"], writes=[("ysb", tt)])
            A("dve", lambda e, tt=tt, xt=xt: e.tensor_tensor(out=xt, in0=ysb[:, tt, :], in1=xt, op=ALU.add),
              reads=[("ysb", tt), rx], writes=[rx])
            dma("sp", x1_d[row0:row0 + 128, :], xt, [rx], [("x1d", row0)], ("x1st", tt % 2))
    P.barrier()
    if stop == 9:
        return nc, st, P

    for g in range(2):
        AR.reset()
        h2T = AR.alloc([16, 512], BF16)
        uT = AR.alloc([64, 512], BF16)
        mark = AR.off
        nctx = norm_setup()
        for tt in range(4):
            row0 = g * 512 + tt * 128
            norm_tile(nctx, x1_d[row0:row0 + 128, :], G2T, S2T,
                      (lambda kc, tt=tt: h2T[:, kc, tt * 128:(tt + 1) * 128]), (("h2T", tt, 0), ("h2T", tt, 1)))
        P.barrier()
        AR.off = mark
        wsf = WStream("wsf", 2, [16, 512])
        rsb = [AR.alloc([512], F32) for _ in range(2)]
        fsb = AR.alloc([4, 2048], F32)
        GT2B = AR.alloc([2048], F32)
        xt7 = [AR.alloc([2048], F32) for _ in range(2)]
        junk7 = AR.alloc([2048], BF16)
        dma("sp", GT2B, gt_d[1].partition_broadcast(128), [], ["GT2B"], "GT2B")
        for fc in range(16):
            w1, rw1 = wsf.load(wsrc(w_ff1, fc * 512, 512))
            for fi in range(4):
                ft = fc * 4 + fi
                bank = 2 + (ft % 2)
                for kc in range(16):
                    mm(PS(bank), w1[:, kc, fi * 128:(fi + 1) * 128], h2T[:, kc, :], kc == 0, kc == 15,
                       reads=[rw1, "h2Tall"], writes=[("ps", bank)])
                rr = rsb[ft % 2]
                A("act", lambda e, bank=bank, rr=rr: e.activation(out=rr, in_=PS(bank), func=AF.Relu),
                  reads=[("ps", bank)], writes=[("rsb", ft % 2)])
                A("dve", lambda e, bank=bank, rr=rr, ft=ft: e.scalar_tensor_tensor(
                    out=uT[:, ft, :], in0=PS(bank), scalar=0.0, in1=rr, op0=ALU.max, op1=ALU.mult),
                  reads=[("ps", bank), ("rsb", ft % 2)], writes=[("uT", ft)])
        for cc in range(4):
            for fq in range(4):
                w2, rw2 = wsf.load(w_ff2[fq * 2048:(fq + 1) * 2048, cc * 512:(cc + 1) * 512].rearrange("(ft p) n -> p ft n", p=128))
                for fi in range(16):
                    ft = fq * 16 + fi
                    for tt in range(4):
                        mm(PS(4 + tt), uT[:, ft, tt * 128:(tt + 1) * 128], w2[:, fi, :], ft == 0, ft == 63,
                           reads=[rw2, ("uT", ft)], writes=[("ps", 4 + tt)])
            for tt in range(4):
                A("act", lambda e, tt=tt, cc=cc: e.activation(out=fsb[:, tt, cc * 512:(cc + 1) * 512], in_=PS(4 + tt), func=AF.Copy),
                  reads=[("ps", 4 + tt)], writes=[("fsb", tt)])
        for tt in range(4):
            row0 = g * 512 + tt * 128
            xt = xt7[tt % 2]
            rx = ("xt7", tt % 2)
            dma("sp", xt, x1_d[row0:row0 + 128, :], [], [rx], rx)
            ss, rss = smcol()
            A("act", lambda e, tt=tt, ss=ss: e.activation(out=junk7, in_=fsb[:, tt, :], func=AF.Square, accum_out=ss),
              reads=[("fsb", tt)], writes=["junk7", rss])
            rs, rrs = rs_from_ss(ss, rss)
            A("dve", lambda e, tt=tt, rs=rs: e.scalar_tensor_tensor(out=fsb[:, tt, :], in0=fsb[:, tt, :], scalar=rs, in1=GT2B,
                                                                     op0=ALU.mult, op1=ALU.mult),
              reads=[("fsb", tt), rrs, "GT2B"], writes=[("fsb", tt)])
            A("dve", lambda e, tt=tt, xt=xt: e.tensor_tensor(out=xt, in0=fsb[:, tt, :], in1=xt, op=ALU.add),
              reads=[("fsb", tt), rx], writes=[rx])
            dma("sp", out_d[row0:row0 + 128, :], xt, [rx], [("outd", row0)], ("ost", tt % 2))
        P.barrier()
    return nc, st, P


def _rope_tables(pos):
    pos = np.asarray(pos, np.float32)
    out = np.zeros((4, 128, pos.shape[0]), np.float32)
    for which, half in ((0, 64), (2, 32)):
        inv = np.power(np.float32(10000.0), -np.arange(half, dtype=np.float32) / np.float32(half)).astype(np.float32)
        ang = (pos[None, :] * inv[:, None]).astype(np.float32)
        c = np.cos(ang).astype(np.float32)
        s = np.sin(ang).astype(np.float32)
        d = np.arange(128)
        dd = d % (2 * half)
        idx = dd % half
        sign = np.where(dd < half, -1.0, 1.0).astype(np.float32)
        out[which] = c[idx]
        out[which + 1] = s[idx] * sign[:, None]
    return out


def _consts(qpos):
    c = np.zeros((128, 5 * 128 + 8 + 32), np.float32)
    c[:, 0:128] = np.eye(128, dtype=np.float32)
    m = np.arange(128)
    p128 = np.zeros((128, 128), np.float32)
    p128[(m + 64) % 128, m] = 1.0
    c[:, 128:256] = p128
    p64 = np.zeros((128, 128), np.float32)
    p64[(m // 64) * 64 + ((m % 64) + 32) % 64, m] = 1.0
    c[:, 256:384] = p64
    k = np.arange(128)[:, None]
    q = np.arange(128)[None, :]
    c[:, 384:512] = (k <= q).astype(np.float32)
    n = np.arange(128) % 16
    c[:, 512:640] = ((n + 1) * 256).astype(np.float32)[None, :]
    c[:, 640:648] = qpos.reshape(8, 128).T
    c[:, 648:680] = (2.0 ** -(np.arange(32, dtype=np.float64) + 1)).astype(np.float32)[None, :]
    return c


def prep_inputs(core, x, c, w_ada, b_ada, g_pre_mix, g_post_mix, w_in, w_moba_out, w_dsa_out, w_o,
                g_pre_ffn, g_post_ffn, w_ff1, w_ff2, shared=None):
    b, j = core // 4, core % 4
    blkA, blkB = j, 7 - j
    own = np.concatenate([np.arange(blkA * 512, blkA * 512 + 512), np.arange(blkB * 512, blkB * 512 + 512)])
    qpos = own.astype(np.float32)
    f = lambda a: np.ascontiguousarray(a, dtype=np.float32)
    tcol = lambda v, n: f(np.asarray(v).reshape(n, 128).T)
    if shared is None:
        shared = {}
    if "w_ada" not in shared:
        esel = np.zeros((128, 16, 128), np.float32)
        for n0 in range(16):
            esel[n0, n0, :] = 1.0
        shared.update(
            w_ada=f(w_ada[0]), b_adaT=tcol(b_ada[0], 96),
            gT=f(np.concatenate([tcol(g_pre_mix[0], 16), tcol(g_post_mix[0], 16),
                                 tcol(g_pre_ffn[0], 16), tcol(g_post_ffn[0], 16)], axis=1)),
            w_in=f(w_in[0]), w_ki2=f(np.concatenate([w_in[0][:, C_KI:C_KI + 64], w_in[0][:, C_KI:C_KI + 64]], axis=1)),
            w_mo=f(w_moba_out[0]), w_do=f(w_dsa_out[0]), w_o=f(w_o[0]), w_ff1=f(w_ff1[0]), w_ff2=f(w_ff2[0]),
            ropeF=_rope_tables(np.arange(T, dtype=np.float32)),
            esel=f(esel.reshape(128, 16 * 128)),
            kposB=f(np.broadcast_to(np.arange(T, dtype=np.float32)[None, :], (128, T))),
        )
    m = dict(shared)
    m.update(
        xfull=f(x[b]), xq=f(x[b][own]), cT=tcol(c[b], 16),
        ropeQ=_rope_tables(qpos), cst=_consts(qpos),
    )
    return m, b, own


_CACHE = {}


def kernel(**inputs):
    inputs = {k: np.asarray(v) for k, v in inputs.items()}
    if "nc" not in _CACHE:
        nc, st, P = build()
        P.emit(st)
        st.close()
        _CACHE["nc"] = nc
    nc = _CACHE["nc"]
    shared = {}
    in_maps, owns = [], []
    for core in range(8):
        m, b, own = prep_inputs(core, shared=shared, **inputs)
        in_maps.append(m)
        owns.append((b, own))
    res = run_bass_kernel_spmd(nc, in_maps, core_ids=list(range(8)))
    out = np.empty((2, T, D), np.float32)
    for core in range(8):
        b, own = owns[core]
        out[b, own] = np.asarray(res.results[core]["out"])
    return out
```
